# Optimizing a Trainium2 kernel written in Bass

```python
import math
import jax, jax.numpy as jnp
from jax import lax
import numpy as np

D_MODEL = 4096
BATCH = 2
SEQ = 4096
DEPTH = 2

CHUNK = 64
Q_BLOCK = 128
ROPE_THETA = 10000.0
EPS = 1e-6
NEG = -1e30

HEAD_DIM = 128
A_HEADS = D_MODEL // 512
A_WIDTH = A_HEADS * HEAD_DIM
IDX_HEADS = 32
IDX_DIM = 64
TOPK_MAX = 256
B_HEADS = D_MODEL // 256
B_NOPE = 128
B_ROPE = 64
B_V = 128
B_QLORA = D_MODEL // 4
B_KVLORA = D_MODEL // 8
B_WIDTH = B_HEADS * B_V
C_HEADS = D_MODEL // 512
C_WIDTH = C_HEADS * HEAD_DIM

MIX_WIDTH = A_WIDTH + B_WIDTH + C_WIDTH
D_FF = 4 * D_MODEL
N_MOD = 6
IN_SPLITS = (A_WIDTH, A_WIDTH, A_WIDTH, IDX_HEADS * IDX_DIM, IDX_DIM, IDX_HEADS,
             B_QLORA, B_KVLORA, B_ROPE,
             C_WIDTH, C_WIDTH, C_WIDTH, C_HEADS)
IN_WIDTH = sum(IN_SPLITS)

kernel_name = 'hybrid_dsa_mla_fox_chunk_encoder'


def _rmsnorm(x, g):
    xf = x.astype(jnp.float32)
    y = xf * lax.rsqrt(jnp.mean(xf * xf, axis=-1, keepdims=True) + EPS)
    return (y * g.astype(jnp.float32)).astype(x.dtype)


def _rope(x, pos):
    d = x.shape[-1]
    inv_freq = jnp.exp(jnp.arange(0, d, 2, dtype=jnp.float32) * (-math.log(ROPE_THETA) / d))
    ang = pos.astype(jnp.float32)[..., None] * inv_freq
    cos = jnp.cos(ang)[:, :, None, :]
    sin = jnp.sin(ang)[:, :, None, :]
    xf = x.astype(jnp.float32)
    x1, x2 = xf[..., : d // 2], xf[..., d // 2:]
    return jnp.concatenate([x1 * cos - x2 * sin, x2 * cos + x1 * sin], axis=-1).astype(x.dtype)


def _to_blocks(a):
    b, s = a.shape[0], a.shape[1]
    return jnp.moveaxis(a.reshape((b, s // Q_BLOCK, Q_BLOCK) + a.shape[2:]), 1, 0)


def _from_blocks(a):
    a = jnp.moveaxis(a, 0, 1)
    return a.reshape((a.shape[0], a.shape[1] * a.shape[2]) + a.shape[3:])


def _dense_attention(q, k, v, log_decay_cum=None):
    s_len, dk = q.shape[1], q.shape[-1]
    n_blk = s_len // Q_BLOCK
    scale = dk ** -0.5
    key_pos = jnp.arange(s_len)
    use_decay = log_decay_cum is not None
    xs = (_to_blocks(q), jnp.arange(n_blk))
    if use_decay:
        xs = xs + (_to_blocks(log_decay_cum),)
        cum_k = jnp.swapaxes(log_decay_cum, 1, 2)

    def block(args):
        q_i, i = args[0], args[1]
        t = i * Q_BLOCK + jnp.arange(Q_BLOCK)
        logits = jnp.einsum('bqhd,bshd->bhqs', q_i, k).astype(jnp.float32) * scale
        if use_decay:
            cum_q = jnp.swapaxes(args[2], 1, 2)
            logits = logits + cum_q[..., None] - cum_k[:, :, None, :]
            mask = key_pos[None, :] <= t[:, None]
        else:
            mask = (key_pos // CHUNK)[None, :] <= (t // CHUNK)[:, None]
        logits = jnp.where(mask, logits, NEG)
        p = jax.nn.softmax(logits, axis=-1)
        return jnp.einsum('bhqs,bshd->bqhd', p.astype(v.dtype), v)

    return _from_blocks(lax.map(block, xs))


def _dsa_attention(q, k, v, iq, ik, iw):
    s_len, d = q.shape[1], q.shape[-1]
    n_blk = s_len // Q_BLOCK
    k_sel = min(TOPK_MAX, s_len // 4)
    kv = jnp.concatenate([k, v], axis=-1)
    key_chunk = jnp.arange(s_len) // CHUNK
    idx_scale = IDX_DIM ** -0.5 * IDX_HEADS ** -0.5
    scale = d ** -0.5

    def block(args):
        q_i, iq_i, iw_i, i = args
        t = i * Q_BLOCK + jnp.arange(Q_BLOCK)
        q_chunk = t // CHUNK
        admissible = key_chunk[None, :] <= q_chunk[:, None]
        rel = jax.nn.relu(jnp.einsum('bqhd,bsd->bqhs', iq_i, ik).astype(jnp.float32))
        score = jnp.einsum('bqhs,bqh->bqs', rel, iw_i.astype(jnp.float32)) * idx_scale
        score = jnp.where(admissible[None], score, NEG)
        _, sel = lax.top_k(score, k_sel)
        kv_sel = jax.vmap(lambda a, ii: a[ii])(kv, sel)
        k_s, v_s = kv_sel[..., :d], kv_sel[..., d:]
        logits = jnp.einsum('bqhd,bqkhd->bhqk', q_i, k_s).astype(jnp.float32) * scale
        valid = (sel // CHUNK) <= q_chunk[None, :, None]
        logits = jnp.where(valid[:, None], logits, NEG)
        p = jax.nn.softmax(logits, axis=-1)
        return jnp.einsum('bhqk,bqkhd->bqhd', p.astype(v.dtype), v_s)

    xs = (_to_blocks(q), _to_blocks(iq), _to_blocks(iw), jnp.arange(n_blk))
    return _from_blocks(lax.map(block, xs))


def _mixer(h, positions, w_in, g_cq, g_ckv, w_uq, w_ukv, b_f, g_out_a, g_out_b, g_out_c, w_out):
    bsz, s_len, _ = h.shape
    proj = h @ w_in
    (qa, ka, va, iq, ik, iw, cq, ckv, kr, qc, kc, vc, fl) = jnp.split(
        proj, np.cumsum(IN_SPLITS)[:-1].tolist(), axis=-1)

    qa = _rope(qa.reshape(bsz, s_len, A_HEADS, HEAD_DIM), positions)
    ka = _rope(ka.reshape(bsz, s_len, A_HEADS, HEAD_DIM), positions)
    va = va.reshape(bsz, s_len, A_HEADS, HEAD_DIM)
    iq = _rope(iq.reshape(bsz, s_len, IDX_HEADS, IDX_DIM), positions)
    ik = _rope(ik.reshape(bsz, s_len, 1, IDX_DIM), positions)[:, :, 0]
    ya = _dsa_attention(qa, ka, va, iq, ik, iw)

    qb = (_rmsnorm(cq, g_cq) @ w_uq).reshape(bsz, s_len, B_HEADS, B_NOPE + B_ROPE)
    qb = jnp.concatenate([qb[..., :B_NOPE], _rope(qb[..., B_NOPE:], positions)], axis=-1)
    kvb = (_rmsnorm(ckv, g_ckv) @ w_ukv).reshape(bsz, s_len, B_HEADS, B_NOPE + B_V)
    kr = _rope(kr.reshape(bsz, s_len, 1, B_ROPE), positions)
    kb = jnp.concatenate([kvb[..., :B_NOPE], jnp.broadcast_to(kr, (bsz, s_len, B_HEADS, B_ROPE))], axis=-1)
    yb = _dense_attention(qb, kb, kvb[..., B_NOPE:])

    log_f = jax.nn.log_sigmoid(fl.astype(jnp.float32) + b_f.astype(jnp.float32))
    cum = jnp.cumsum(log_f, axis=1)
    yc = _dense_attention(qc.reshape(bsz, s_len, C_HEADS, HEAD_DIM),
                          kc.reshape(bsz, s_len, C_HEADS, HEAD_DIM),
                          vc.reshape(bsz, s_len, C_HEADS, HEAD_DIM),
                          log_decay_cum=cum)

    y = jnp.concatenate([
        _rmsnorm(ya.reshape(bsz, s_len, A_WIDTH), g_out_a),
        _rmsnorm(yb.reshape(bsz, s_len, B_WIDTH), g_out_b),
        _rmsnorm(yc.reshape(bsz, s_len, C_WIDTH), g_out_c)], axis=-1)
    return y @ w_out


def setup_inputs(seed: int = 0) -> dict:
    key = jax.random.key(seed)
    ks = jax.random.split(key, 24)
    f32 = jnp.float32

    def nrm(k, shape, fan_in):
        return jax.random.normal(k, shape, f32) * fan_in ** -0.5

    def gain(k, shape):
        return 1.0 + 0.02 * jax.random.normal(k, shape, f32)

    x = jax.random.normal(ks[0], (BATCH, SEQ, D_MODEL), f32)
    c = jax.random.normal(ks[1], (BATCH, D_MODEL), f32)
    offset = jax.random.randint(ks[2], (BATCH, 1), 0, 64, dtype=jnp.int32) * CHUNK
    positions = offset + jnp.arange(SEQ, dtype=jnp.int32)[None, :]
    return {
        'x': x,
        'c': c,
        'positions': positions,
        'w_ada': nrm(ks[3], (DEPTH, D_MODEL, N_MOD * D_MODEL), D_MODEL),
        'b_ada': 0.02 * jax.random.normal(ks[4], (DEPTH, N_MOD * D_MODEL), f32),
        'g_attn': gain(ks[5], (DEPTH, D_MODEL)),
        'w_in': nrm(ks[6], (DEPTH, D_MODEL, IN_WIDTH), D_MODEL),
        'g_cq': gain(ks[7], (DEPTH, B_QLORA)),
        'g_ckv': gain(ks[8], (DEPTH, B_KVLORA)),
        'w_uq': nrm(ks[9], (DEPTH, B_QLORA, B_HEADS * (B_NOPE + B_ROPE)), B_QLORA),
        'w_ukv': nrm(ks[10], (DEPTH, B_KVLORA, B_HEADS * (B_NOPE + B_V)), B_KVLORA),
        'b_f': 2.0 + 2.0 * jax.random.uniform(ks[11], (DEPTH, C_HEADS), f32),
        'g_out_a': gain(ks[12], (DEPTH, A_WIDTH)),
        'g_out_b': gain(ks[13], (DEPTH, B_WIDTH)),
        'g_out_c': gain(ks[14], (DEPTH, C_WIDTH)),
        'w_out': nrm(ks[15], (DEPTH, MIX_WIDTH, D_MODEL), MIX_WIDTH),
        'g_mlp': gain(ks[16], (DEPTH, D_MODEL)),
        'w_up': nrm(ks[17], (DEPTH, D_MODEL, D_FF), D_MODEL),
        'w_down': nrm(ks[18], (DEPTH, D_FF, D_MODEL), D_FF),
        'g_final': gain(ks[19], (D_MODEL,)),
    }


def reference(x, c, positions, w_ada, b_ada, g_attn, w_in, g_cq, g_ckv, w_uq, w_ukv, b_f,
              g_out_a, g_out_b, g_out_c, w_out, g_mlp, w_up, w_down, g_final):
    cond = jax.nn.silu(c)
    for l in range(DEPTH):
        mod = (cond @ w_ada[l] + b_ada[l])[:, None, :]
        sh1, sc1, gt1, sh2, sc2, gt2 = jnp.split(mod, N_MOD, axis=-1)
        h = _rmsnorm(x, g_attn[l]) * (1 + sc1) + sh1
        x = x + gt1 * _mixer(h, positions, w_in[l], g_cq[l], g_ckv[l], w_uq[l], w_ukv[l], b_f[l],
                             g_out_a[l], g_out_b[l], g_out_c[l], w_out[l])
        h = _rmsnorm(x, g_mlp[l]) * (1 + sc2) + sh2
        x = x + gt2 * (jnp.square(jax.nn.relu(h @ w_up[l])) @ w_down[l])
    return _rmsnorm(x, g_final)
```

```python
import numpy as np
import concourse.bass as bass
import concourse.mybir as mybir
from concourse.bass_utils import run_bass_kernel_spmd

F32 = mybir.dt.float32
BF16 = mybir.dt.bfloat16
I32 = mybir.dt.int32
AF = mybir.ActivationFunctionType
ALU = mybir.AluOpType

NC = 8
D = 4096
T = 1024
TB = 512
KC = 32
DFF = 16384
INW = 9896
EPS = 1e-6
NEGB = -30000.0
import os
sys_argv_jobs = [int(os.environ.get("MKJOBS", "99"))]
ARENA_BYTES = 206000

G_KA, G_IK, G_KR, G_KB, G_KCC, G_ROWS = 0, 1024, 1088, 1152, 3200, 4224
G_VA, G_VB, G_VC, GV_ROWS = 0, 1024, 3072, 4096


class Prog:
    def __init__(self, nc):
        self.nc = nc
        self.E = {"pe": nc.tensor, "act": nc.scalar, "dve": nc.vector, "pool": nc.gpsimd, "sp": nc.sync}
        self.sem = {}
        self.cnt = {}
        for e in ("pe", "act", "dve", "pool"):
            self.sem[e] = nc.alloc_semaphore("s_" + e)
            self.cnt[e] = 0
        self.known = {e: {} for e in self.E}
        self.lastw = {}
        self.reads = {}
        self.nwaits = 0
        self.nops = 0
        self.noflush = set()
        self.trace = {e: [] for e in self.E}

    def simulate(self):
        val = {s: 0 for s in self.sem}
        pc = {e: 0 for e in self.E}
        prog = True
        while prog:
            prog = False
            for e in self.E:
                tr = self.trace[e]
                while pc[e] < len(tr):
                    kind, s, c, tag = tr[pc[e]]
                    if kind == "wait":
                        if val[s] < c:
                            break
                    else:
                        val[s] += c
                    pc[e] += 1
                    prog = True
        stuck = {e: (pc[e], len(self.trace[e]), self.trace[e][pc[e]] if pc[e] < len(self.trace[e]) else None) for e in self.E}
        return stuck, val

    def _dsem(self, key):
        k = ("dma", key)
        if k not in self.sem:
            self.sem[k] = self.nc.alloc_semaphore("d_%d" % len(self.sem))
            self.cnt[k] = 0
        return k

    def _wait(self, eng, deps):
        need = {}
        for (s, c) in deps:
            if s == eng and eng == "pe":
                continue
            if isinstance(s, tuple):
                c = self.cnt[s]
            if c > need.get(s, 0):
                need[s] = c
        kn = self.known[eng]
        for s, c in need.items():
            if kn.get(s, 0) >= c:
                continue
            self.E[eng].wait_ge(self.sem[s], c)
            self.trace[eng].append(("wait", s, c, None))
            kn[s] = c
            self.nwaits += 1

    def _deps(self, reads, writes):
        deps = []
        for k in reads:
            t = self.lastw.get(k)
            if t is not None:
                deps.append(t)
        for k in writes:
            t = self.lastw.get(k)
            if t is not None:
                deps.append(t)
            deps.extend(self.reads.get(k, ()))
        return deps

    def _commit(self, tok, reads, writes):
        for k in writes:
            self.lastw[k] = tok
            self.reads[k] = []
        for k in reads:
            lst = self.reads.setdefault(k, [])
            lst[:] = [t for t in lst if t[0] != tok[0]]
            lst.append(tok)

    def op(self, eng, fn, reads=(), writes=()):
        self._wait(eng, self._deps(reads, writes))
        ins = fn(self.E[eng])
        self.cnt[eng] += 1
        ins.then_inc(self.sem[eng], 1)
        self.trace[eng].append(("inc", eng, 1, (tuple(reads), tuple(writes))))
        self._commit((eng, self.cnt[eng]), reads, writes)
        self.nops += 1
        return ins

    def dma(self, q, out, in_, reads=(), writes=(), key=None):
        self._wait(q, self._deps(reads, writes))
        sk = self._dsem(key if key is not None else (tuple(writes), q))
        ins = self.E[q].dma_start(out=out, in_=in_)
        self.cnt[sk] += 16
        ins.then_inc(self.sem[sk], 16)
        self.trace[q].append(("inc", sk, 16, (tuple(reads), tuple(writes))))
        self._commit((sk, self.cnt[sk]), reads, writes)
        self.nops += 1
        return ins

    def collective(self, fn, name, reads=(), writes=()):
        self._wait("pool", self._deps(reads, writes))
        sk = self._dsem(("cc", name))
        ins = fn(self.E["pool"])
        self.cnt[sk] += 1
        ins.then_inc(self.sem[sk])
        self.trace["pool"].append(("inc", sk, 1, (tuple(reads), tuple(writes))))
        self._commit((sk, 1), reads, writes)
        self.noflush.add(sk) if name.startswith("w_") else None
        return ins

    def finish(self, keys, eng="sp"):
        self._wait(eng, [self.lastw[k] for k in keys if k in self.lastw])

    def barrier_all(self):
        allt = [(s, c) for s, c in self.cnt.items() if c > 0 and s not in self.noflush]
        for e in self.E:
            self._wait(e, [t for t in allt if t[0] != e])


class Ctx:
    pass


def wnames_for(stage):
    if stage < 0.5:
        return []
    n = ["w_in", "w_uq", "w_ukv"]
    if stage >= 5:
        n.append("w_out")
    if stage >= 6:
        n.append("w_up")
    if stage >= 7:
        n.append("w_down")
    return n


def _dts(dt):
    return 4 if dt in (F32, I32) else 2


def build(NL=2, stage=99, dbg=()):
    nc = bass.Bass("TRN2", target_bir_lowering=False)
    P = Prog(nc)
    K = Ctx()
    K.nc, K.P = nc, P

    def din(name, shape, dt=F32):
        return nc.dram_tensor(name, list(shape), dt, kind="ExternalInput").ap()

    def dint(name, shape, dt):
        return nc.dram_tensor(name, list(shape), dt)

    xT_in = din("xT_in", [D, T])
    pos_in = din("pos", [1, T], I32)
    cT_in = din("cT", [128, 64])
    wada_in = din("wada", [NL, D, 3072])
    bada_in = din("bada", [128, NL * 24])
    gvec_in = din("gvec", [128, 512])
    bfrow_in = din("bfrow", [128, NL * 8])
    cst_in = din("cst", [128, 4 * 128 + 4 + 8])
    maskB_in = din("maskB", [128, 2 * 8 * 128])
    maskQ_in = din("maskQ", [128, 8 * 128])
    wshape = {"w_in": (D, INW), "w_uq": (1024, 3072), "w_ukv": (512, 4096), "w_out": (D, D),
              "w_up": (D, DFF), "w_down": (DFF, D)}
    wsh = {n: din(n, [NL, wshape[n][0] // 8, wshape[n][1]]) for n in wnames_for(stage)}
    outT = nc.dram_tensor("outT", [D, T], F32, kind="ExternalOutput").ap()
    dbg_out = {}

    xT = dint("xT", [D, T], F32)
    gmod_in = dint("gmod_in", [128, NL * 48], F32)
    gmod_out = dint("gmod_out", [8 * 128, NL * 48], F32)
    wg_in = {(n, l): dint("wgi_%s_%d" % (n, l), [wshape[n][0] // 8, wshape[n][1]], BF16) for n in wsh for l in range(NL)}
    wfull = {(n, l): dint("wf_%s_%d" % (n, l), [wshape[n][0], wshape[n][1]], BF16) for n in wsh for l in range(NL)}
    qaT = dint("qaT", [1024, T], BF16)
    iqT = dint("iqT", [2048, T], BF16)
    iw_d = dint("iw_d", [T, 32], F32)
    cqT_d = dint("cqT_d", [1024, T], F32)
    ckvT_d = dint("ckvT_d", [512, T], F32)
    qbnT = dint("qbnT", [2048, T], BF16)
    qbrT = dint("qbrT", [1024, T], BF16)
    qcT = dint("qcT", [1024, T], BF16)
    gk_in = dint("gk_in", [G_ROWS, T], BF16)
    gk_out = dint("gk_out", [8 * G_ROWS, T], BF16)
    gv_in = dint("gv_in", [GV_ROWS, T], BF16)
    gv_out = dint("gv_out", [8 * GV_ROWS, T], BF16)
    gf_in_t = dint("gf_in", [8, T], F32)
    gf_out_t = dint("gf_out", [64, T], F32)
    gf_in = gf_in_t[:, :].rearrange("a b -> (a b)").rearrange("(t h) -> t h", h=8)
    gf_out = gf_out_t[:, :].rearrange("a b -> (a b)").rearrange("(t h) -> t h", h=8)
    uT = dint("uT", [DFF, T], BF16)

    arena = nc.alloc_sbuf_tensor("arena", [128, ARENA_BYTES // 2], BF16)
    abase = nc.lookup_mloc(arena).addr
    K.pptr = 0
    K.wbase = None
    K.wptr = 0
    K.uid = 0

    def sb(name, shape, dt, work=True):
        nb = _dts(dt)
        for s in shape[1:]:
            nb *= s
        nb = (nb + 63) // 64 * 64
        K.uid += 1
        if work:
            off = K.wbase + K.wptr
            K.wptr += nb
        else:
            assert K.wbase is None
            off = K.pptr
            K.pptr += nb
        assert off + nb <= ARENA_BYTES, (name, off, nb)
        return nc.alloc_sbuf_tensor_at("%s_%d" % (name, K.uid), list(shape), dt, offset=abase + off)

    def work_reset():
        if K.wbase is None:
            K.wbase = K.pptr
        K.wptr = 0

    ps = [nc.alloc_psum_tensor("ps%d" % i, [128, 512], F32) for i in range(8)]
    psk = [("ps", i) for i in range(8)]

    actT = sb("actT", [128, KC, T], BF16, work=False)
    cosT = [sb("cos%d" % v, [128, T], F32, work=False) for v in range(2)]
    sinT = [sb("sin%d" % v, [128, T], F32, work=False) for v in range(2)]
    identf = sb("identf", [128, 128], F32, work=False)
    identb = sb("identb", [128, 128], BF16, work=False)
    Rb = [sb("R%d" % v, [128, 128], BF16, work=False) for v in range(2)]
    onesf = sb("onesf", [128, 128], F32, work=False)
    onesb = sb("onesb", [128, 128], BF16, work=False)
    triu = sb("triu", [128, 128], F32, work=False)
    rtab = sb("rtab", [128, 4], F32, work=False)
    selt = sb("selt", [128, 8], F32, work=False)
    gvec = sb("gvec", [128, 512], F32, work=False)
    bfrow = sb("bfrow", [128, NL * 8], F32, work=False)
    bada = sb("bada", [128, NL * 24], F32, work=False)
    modT = sb("modT", [128, 8, NL * 48], F32, work=False)
    s1T = sb("s1T", [128, NL * 2 * KC * 2], F32, work=False)
    maskB = sb("maskB", [128, 2, 8, 128], BF16, work=False)
    maskQ = sb("maskQ", [128, 8, 128], F32, work=False)
    condT = sb("condT", [128, 64], F32, work=False)
    epst_p = sb("epst_p", [128, 1], F32, work=False)
    work_reset()

    GV_ATTN, GV_MLP, GV_FIN, GV_CQ, GV_CKV, GV_OUT = 0, 64, 128, 160, 176, 184

    def mod_ap(l, k, fc, b=None):
        c = k * 32 + fc
        r, cj = c // 24, c % 24
        col = (l * 24 + cj) * 2
        if b is None:
            return modT[:, r, col:col + 2]
        return modT[:, r, col + b:col + b + 1]

    def s_ap(l, which, fc, b):
        o = ((l * 2 + which) * KC + fc) * 2 + b
        return s1T[:, o:o + 1]

    P.dma("sp", identf[:], cst_in[:, 0:128], writes=["identf"])
    P.dma("pool", identb[:], cst_in[:, 0:128], writes=["identb"])
    P.dma("pool", Rb[0][:], cst_in[:, 128:256], writes=["R0"])
    P.dma("pool", Rb[1][:], cst_in[:, 256:384], writes=["R1"])
    P.dma("sp", triu[:], cst_in[:, 384:512], writes=["triu"])
    P.dma("sp", rtab[:], cst_in[:, 512:516], writes=["rtab"])
    P.dma("sp", selt[:], cst_in[:, 516:524], writes=["selt"])
    P.dma("sp", gvec[:], gvec_in, writes=["gvec"])
    P.dma("sp", bfrow[:], bfrow_in, writes=["bfrow"])
    P.dma("sp", bada[:], bada_in, writes=["bada"])
    P.dma("pool", maskB[:].rearrange("p a b c -> p (a b c)"), maskB_in, writes=["maskB"])
    P.dma("sp", maskQ[:].rearrange("p a b -> p (a b)"), maskQ_in, writes=["maskQ"])
    P.dma("sp", condT[:], cT_in, writes=["condT"])
    P.op("dve", lambda e: e.memset(epst_p[:], EPS), writes=["epst"])
    P.op("dve", lambda e: e.memset(onesf[:], 1.0), writes=["onesf"])
    P.op("dve", lambda e: e.memset(onesb[:], 1.0), writes=["onesb"])
    P.op("act", lambda e: e.activation(out=condT[:], in_=condT[:], func=AF.Silu), reads=["condT"], writes=["condT"])

    def weight_gather(n, l):
        src = wsh[n][l]
        rows, cols = wshape[n][0] // 8, wshape[n][1]
        dst = wg_in[(n, l)]
        step = max(1, (1 << 21) // cols)
        r0 = 0
        while r0 < rows:
            r1 = min(rows, r0 + step)
            P.dma("pool", dst[r0:r1, :], src[r0:r1, :], writes=[("wgi", n, l)], key=("wgi", n, l))
            r0 = r1
        P.collective(lambda e: e.collective_compute("AllGather", ALU.bypass, replica_groups=[list(range(NC))],
                                                    ins=[dst.ap().opt()], outs=[wfull[(n, l)].ap().opt()]),
                     "w_%s_%d" % (n, l), reads=[("wgi", n, l)], writes=[("wf", n, l)])

    wa = [sb("wa%d" % i, [128, KC, 128], F32) for i in range(2)]
    modloc = sb("modloc", [128, NL * 48], F32)
    for l in range(NL):
        for cj in range(24):
            s = (l * 24 + cj) % 2
            wv = wada_in[l][:, cj * 128:(cj + 1) * 128].rearrange("(kc p) n -> p kc n", p=128)
            for q in range(4):
                P.dma("sp", wa[s][:, q * 8:(q + 1) * 8, :], wv[:, q * 8:(q + 1) * 8, :], writes=[("wa", s)], key=("wa", s))
            bk = s
            for kc in range(KC):
                P.op("pe", lambda e: e.matmul(ps[bk][:, 0:2], wa[s][:, kc, :], condT[:, 2 * kc:2 * kc + 2],
                                              start=(kc == 0), stop=(kc == KC - 1)),
                     reads=[("wa", s), "condT"], writes=[psk[bk]])
            col = (l * 24 + cj) * 2
            P.op("act", lambda e: e.activation(out=modloc[:, col:col + 2], in_=ps[bk][:, 0:2], func=AF.Identity,
                                               bias=bada[:, l * 24 + cj:l * 24 + cj + 1], scale=1.0),
                 reads=[psk[bk], "bada"], writes=["modloc"])
    P.dma("sp", gmod_in[:, :], modloc[:], reads=["modloc"], writes=["gmod_in"])
    P.collective(lambda e: e.collective_compute("AllGather", ALU.bypass, replica_groups=[list(range(NC))],
                                                ins=[gmod_in.ap().opt()], outs=[gmod_out.ap().opt()]),
                 "mod", reads=["gmod_in"], writes=["gmod_out"])
    order = []
    for l in range(NL):
        for n in wnames_for(stage):
            order.append((n, l))
    for (n, l) in order:
        weight_gather(n, l)
    P.dma("sp", modT[:], gmod_out[:, :].rearrange("(r p) c -> p r c", p=128), reads=["gmod_out"], writes=["modT"])
    for l in range(NL):
        for which in range(2):
            for fc in range(KC):
                gcol = (GV_ATTN if which == 0 else GV_MLP) + l * 32 + fc
                o = ((l * 2 + which) * KC + fc) * 2
                P.op("dve", lambda e: e.tensor_scalar(out=s1T[:, o:o + 2], in0=mod_ap(l, 1 + 3 * which, fc), scalar1=1.0,
                                                      scalar2=gvec[:, gcol:gcol + 1], op0=ALU.add, op1=ALU.mult),
                     reads=["modT", "gvec"], writes=["s1T"])

    posb = sb("posb", [128, T], I32)
    posf = sb("posf", [128, T], F32)
    tq = sb("tq", [128, T], F32)
    tki = sb("tki", [128, T], I32)
    tkf = sb("tkf", [128, T], F32)
    P.dma("sp", posb[:], pos_in.partition_broadcast(128), writes=["posb"])
    P.op("dve", lambda e: e.tensor_copy(out=posf[:], in_=posb[:]), reads=["posb"], writes=["posf"])
    for v in range(2):
        for (tab, off) in ((sinT[v], 0.0), (cosT[v], 0.25)):
            P.op("dve", lambda e: e.tensor_scalar(out=tq[:], in0=posf[:], scalar1=rtab[:, v:v + 1], scalar2=off,
                                                  op0=ALU.mult, op1=ALU.add), reads=["posf", "rtab"], writes=["tq"])
            P.op("dve", lambda e: e.tensor_copy(out=tki[:], in_=tq[:]), reads=["tq"], writes=["tki"])
            P.op("dve", lambda e: e.tensor_copy(out=tkf[:], in_=tki[:]), reads=["tki"], writes=["tkf"])
            P.op("dve", lambda e: e.tensor_tensor(out=tq[:], in0=tq[:], in1=tkf[:], op=ALU.subtract),
                 reads=["tq", "tkf"], writes=["tq"])
            P.op("act", lambda e: e.activation(out=tab[:], in_=tq[:], func=AF.Sin, scale=6.28318),
                 reads=["tq"], writes=[("rt", v)])
        P.op("dve", lambda e: e.tensor_scalar(out=sinT[v][:], in0=sinT[v][:], scalar1=rtab[:, 2 + v:3 + v], scalar2=None,
                                              op0=ALU.mult), reads=[("rt", v), "rtab"], writes=[("rt", v)])

    if "mod" in dbg:
        d = nc.dram_tensor("dbg_mod", [128, 8 * NL * 48], F32, kind="ExternalOutput").ap()
        P.dma("sp", d, modT[:].rearrange("p a b -> p (a b)"), reads=["modT"], writes=["dbg_mod"])
        dbg_out["dbg_mod"] = 1
        d = nc.dram_tensor("dbg_rope", [128, 4 * T], F32, kind="ExternalOutput").ap()
        for i, tt in enumerate((cosT[0], sinT[0], cosT[1], sinT[1])):
            P.dma("sp", d[:, i * T:(i + 1) * T], tt[:], reads=[("rt", i // 2)], writes=["dbg_rope%d" % i])
        dbg_out["dbg_rope"] = 1

    K.bank = 0

    def nextbank(n=4):
        b = K.bank
        K.bank = (K.bank + 1) % n
        return b

    def rmsnorm_stats(src_chunk_loader, nch, n_feat, rstd, tb, xs, sq):
        for c in range(nch):
            sl = c % 2
            k = src_chunk_loader(c, sl, tb)
            P.op("act", lambda e: e.activation(out=sq[sl][:], in_=xs[sl][:], func=AF.Square), reads=[k], writes=[("sq", sl)])
            P.op("pe", lambda e: e.matmul(ps[6][:, :], onesf[:], sq[sl][:], start=(c == 0), stop=(c == nch - 1)),
                 reads=[("sq", sl), "onesf"], writes=[psk[6]])
        P.op("act", lambda e: e.activation(out=rstd[:], in_=ps[6][:, :], func=AF.Sqrt, bias=K.epst[:, 0:1], scale=1.0 / n_feat),
             reads=[psk[6], "epst"], writes=[("rstd", id(rstd))])
        P.op("dve", lambda e: e.reciprocal(out=rstd[:], in_=rstd[:]), reads=[("rstd", id(rstd))], writes=[("rstd", id(rstd))])

    K.epst = epst_p

    def load_w_tile(wf_key, wfd, col0, ncols, kcn, wt, slot, kc0=0, q="sp"):
        v = wfd[kc0 * 128:(kc0 + kcn) * 128, col0:col0 + ncols].rearrange("(kc p) n -> p kc n", p=128)
        st = 8
        for a in range(0, kcn, st):
            b = min(kcn, a + st)
            P.dma(q, wt[:, a:b, 0:ncols], v[:, a:b, :], reads=[wf_key], writes=[("wb", slot)], key=("wb", slot))

    def run_jobs(X, xkeys, kcn, jobs, wbuf):
        nslot = len(wbuf)

        def pre(i):
            if i < len(jobs):
                wk, wfd, col0, ncols, _ = jobs[i]
                load_w_tile(wk, wfd, col0, ncols, kcn, wbuf[i % nslot], i % nslot)
        pre(0)
        pre(1)
        for i, (wk, wfd, col0, ncols, subs) in enumerate(jobs):
            pre(i + 2)
            wt = wbuf[i % nslot]
            wkey = ("wb", i % nslot)
            for (kind, c0, n, handler) in subs:
                if kind == "fm":
                    for tb in range(2):
                        bk = nextbank()
                        for kc in range(kcn):
                            P.op("pe", lambda e: e.matmul(ps[bk][0:n, :], wt[:, kc, c0:c0 + n], X[:, kc, tb * TB:(tb + 1) * TB],
                                                          start=(kc == 0), stop=(kc == kcn - 1)),
                                 reads=[wkey, xkeys[kc]], writes=[psk[bk]])
                        handler(bk, n, tb)
                else:
                    for tt in range(8):
                        bk = nextbank()
                        for kc in range(kcn):
                            P.op("pe", lambda e: e.matmul(ps[bk][:, 0:n], X[:, kc, tt * 128:(tt + 1) * 128], wt[:, kc, c0:c0 + n],
                                                          start=(kc == 0), stop=(kc == kcn - 1)),
                                 reads=[wkey, xkeys[kc]], writes=[psk[bk]])
                        handler(bk, n, tt)

    actk = [("actT", kc) for kc in range(KC)]

    def norm_modulate(l, which, xsrc, xkey):
        work_reset()
        xs = [sb("xs%d" % i, [128, TB], F32) for i in range(2)]
        sq = [sb("sq%d" % i, [128, TB], F32) for i in range(2)]
        rstd = [sb("rstd%d" % i, [128, TB], F32) for i in range(2)]
        tmp = [sb("ntmp%d" % i, [128, TB], F32) for i in range(2)]

        def loader(c, sl, tb):
            P.dma("sp", xs[sl][:], xsrc[c * 128:(c + 1) * 128, tb * TB:(tb + 1) * TB], reads=[xkey], writes=[("xs", sl)],
                  key=("xs", sl))
            return ("xs", sl)
        for tb in range(2):
            rmsnorm_stats(loader, KC, D, rstd[tb], tb, xs, sq)
        for tb in range(2):
            for c in range(KC):
                sl = c % 2
                k = loader(c, sl, tb)
                P.op("dve", lambda e: e.tensor_tensor(out=tmp[sl][:], in0=xs[sl][:], in1=rstd[tb][:], op=ALU.mult),
                     reads=[k, ("rstd", id(rstd[tb]))], writes=[("ntmp", sl)])
                P.op("act", lambda e: e.activation(out=actT[:, c, tb * TB:(tb + 1) * TB], in_=tmp[sl][:], func=AF.Identity,
                                                   bias=mod_ap(l, 3 * which, c, tb), scale=s_ap(l, which, c, tb)),
                     reads=[("ntmp", sl), "modT", "s1T"], writes=[actk[c]])

    def phase1(l):
        xsrc = xT_in if l == 0 else xT
        norm_modulate(l, 0, xsrc, "xT")
        P.barrier_all()
        if stage < 1.2:
            return
        work_reset()
        wbuf = [sb("wbuf%d" % i, [128, KC, 320], BF16) for i in range(3)]
        stf = [sb("stf%d" % i, [128, TB], F32) for i in range(4)]
        stb = [sb("stb%d" % i, [128, TB], BF16) for i in range(4)]
        xb = [sb("xb%d" % i, [128, TB], BF16) for i in range(2)]
        t1 = [sb("t1%d" % i, [128, TB], F32) for i in range(2)]
        t2 = [sb("t2%d" % i, [128, TB], F32) for i in range(2)]
        K.si = 0

        def slot(n=4):
            K.si += 1
            return K.si % n

        def h_store(dst, row0, scale=1.0, dt=BF16, key="q1"):
            def h(bk, n, tb):
                s = slot()
                P.op("act", lambda e: e.activation(out=stf[s][0:n, :], in_=ps[bk][0:n, :], func=AF.Identity, scale=scale),
                     reads=[psk[bk]], writes=[("stf", s)])
                if dt == BF16:
                    P.op("dve", lambda e: e.tensor_copy(out=stb[s][0:n, :], in_=stf[s][0:n, :]), reads=[("stf", s)], writes=[("stb", s)])
                    st, sk = stb[s], ("stb", s)
                else:
                    st, sk = stf[s], ("stf", s)
                P.dma("sp", dst[row0:row0 + n, tb * TB:(tb + 1) * TB], st[0:n, :], reads=[sk], writes=[key], key=("st", key))
            return h

        def h_rope(dst, row0, v, scale=1.0, key="q1"):
            def h(bk, n, tb):
                s2 = slot(2)
                s4 = slot()
                rb = 4 + s2
                P.op("act", lambda e: e.activation(out=t1[s2][0:n, :], in_=ps[bk][0:n, :], func=AF.Identity, scale=scale),
                     reads=[psk[bk]], writes=[("t1", s2)])
                P.op("dve", lambda e: e.tensor_copy(out=xb[s2][0:n, :], in_=t1[s2][0:n, :]), reads=[("t1", s2)], writes=[("xb", s2)])
                P.op("pe", lambda e: e.matmul(ps[rb][0:n, :], Rb[v][0:n, 0:n], xb[s2][0:n, :], start=True, stop=True),
                     reads=[("xb", s2), "R%d" % v], writes=[psk[rb]])
                P.op("act", lambda e: e.activation(out=t2[s2][0:n, :], in_=ps[rb][0:n, :], func=AF.Identity),
                     reads=[psk[rb]], writes=[("t2", s2)])
                P.op("dve", lambda e: e.tensor_tensor(out=t1[s2][0:n, :], in0=t1[s2][0:n, :], in1=cosT[v][0:n, tb * TB:(tb + 1) * TB], op=ALU.mult),
                     reads=[("t1", s2), ("rt", v)], writes=[("t1", s2)])
                P.op("dve", lambda e: e.tensor_tensor(out=t2[s2][0:n, :], in0=t2[s2][0:n, :], in1=sinT[v][0:n, tb * TB:(tb + 1) * TB], op=ALU.mult),
                     reads=[("t2", s2), ("rt", v)], writes=[("t2", s2)])
                P.op("dve", lambda e: e.tensor_tensor(out=stb[s4][0:n, :], in0=t1[s2][0:n, :], in1=t2[s2][0:n, :], op=ALU.add),
                     reads=[("t1", s2), ("t2", s2)], writes=[("stb", s4)])
                P.dma("sp", dst[row0:row0 + n, tb * TB:(tb + 1) * TB], stb[s4][0:n, :], reads=[("stb", s4)], writes=[key],
                      key=("st", key))
            return h

        def h_tm(dst, col0, dt=BF16, key="q1"):
            def h(bk, n, tt):
                s = slot()
                P.op("act", lambda e: e.activation(out=stf[s][:, 0:n], in_=ps[bk][:, 0:n], func=AF.Identity), reads=[psk[bk]], writes=[("stf", s)])
                if dt == BF16:
                    P.op("dve", lambda e: e.tensor_copy(out=stb[s][:, 0:n], in_=stf[s][:, 0:n]), reads=[("stf", s)], writes=[("stb", s)])
                    st, sk = stb[s], ("stb", s)
                else:
                    st, sk = stf[s], ("stf", s)
                P.dma("sp", dst[tt * 128:(tt + 1) * 128, col0:col0 + n], st[:, 0:n], reads=[sk], writes=[key], key=("st", key))
            return h

        def h_fl(bk, n, tt):
            s = slot()
            st = stf[s]
            sk = ("stf", s)
            P.op("act", lambda e: e.activation(out=st[:, 0:8], in_=ps[bk][:, 0:8], func=AF.Identity), reads=[psk[bk]], writes=[sk])
            P.op("dve", lambda e: e.tensor_tensor(out=st[:, 0:8], in0=st[:, 0:8], in1=bfrow[:, l * 8:(l + 1) * 8], op=ALU.add),
                 reads=[sk, "bfrow"], writes=[sk])
            P.op("act", lambda e: e.activation(out=st[:, 0:8], in_=st[:, 0:8], func=AF.Exp, scale=-1.0), reads=[sk], writes=[sk])
            P.op("act", lambda e: e.activation(out=st[:, 0:8], in_=st[:, 0:8], func=AF.Ln, bias=1.0, scale=1.0), reads=[sk], writes=[sk])
            P.op("dve", lambda e: e.tensor_scalar(out=st[:, 0:8], in0=st[:, 0:8], scalar1=-1.0, scalar2=None, op0=ALU.mult),
                 reads=[sk], writes=[sk])
            P.dma("sp", gf_in[tt * 128:(tt + 1) * 128, :], st[:, 0:8], reads=[sk], writes=["gf_in"], key=("st", "gf"))

        va_d = gv_in[G_VA:G_VA + 1024, :].rearrange("a b -> (a b)").rearrange("(t f) -> t f", f=1024)
        vb_d = gv_in[G_VB:G_VB + 2048, :].rearrange("a b -> (a b)").rearrange("(t f) -> t f", f=2048)
        vc_d = gv_in[G_VC:G_VC + 1024, :].rearrange("a b -> (a b)").rearrange("(t f) -> t f", f=1024)
        wk = ("wf", "w_in", l)
        wfd = wfull[("w_in", l)]
        sA = 128.0 ** -0.5
        jobs = []

        def fm_jobs(c0, width, mk):
            for j in range(0, width, 256):
                subs = []
                for m in range(0, 256, 128):
                    subs.append(("fm", m, 128, mk(j + m)))
                jobs.append((wk, wfd, c0 + j, 256, subs))

        def tm_jobs(c0, width, dst):
            for j in range(0, width, 256):
                jobs.append((wk, wfd, c0 + j, 256, [("tm", 0, 256, h_tm(dst, j, key="gk"))]))
        fm_jobs(5216, 1024, lambda r: h_store(cqT_d, r, dt=F32, key="cq"))
        fm_jobs(6240, 512, lambda r: h_store(ckvT_d, r, dt=F32, key="ckv"))
        fm_jobs(0, 1024, lambda r: h_rope(qaT, r, 0, sA))
        fm_jobs(1024, 1024, lambda r: h_rope(gk_in, G_KA + r, 0, key="gk"))
        tm_jobs(2048, 1024, va_d)
        fm_jobs(3072, 2048, lambda r: h_rope(iqT, r, 1))
        jobs.append((wk, wfd, 5120, 96, [("fm", 0, 64, h_rope(gk_in, G_IK, 1, key="gk")),
                                         ("tm", 64, 32, h_tm(iw_d, 0, dt=F32))]))
        jobs.append((wk, wfd, 6752, 64, [("fm", 0, 64, h_rope(gk_in, G_KR, 1, key="gk"))]))
        fm_jobs(6816, 1024, lambda r: h_store(qcT, r, sA))
        fm_jobs(7840, 1024, lambda r: h_store(gk_in, G_KCC + r, key="gk"))
        tm_jobs(8864, 768, vc_d)
        jobs.append((wk, wfd, 8864 + 768, 264, [("tm", 0, 256, h_tm(vc_d, 768, key="gk")), ("tm", 256, 8, h_fl)]))
        if stage < 1.3:
            jobs = jobs[int(os.environ.get("MKSKIP", "0")):int(sys_argv_jobs[0])]
        run_jobs(actT, actk, KC, jobs, wbuf)
        if stage < 1.4:
            P.barrier_all()
            return

        xs = [sb("cxs%d" % i, [128, TB], F32) for i in range(2)]
        sq = [sb("csq%d" % i, [128, TB], F32) for i in range(2)]
        rstd = [sb("crstd%d" % i, [128, TB], F32) for i in range(4)]
        for gi, (srcd, nch, c_off, gcol, key) in enumerate(((cqT_d, 8, 0, GV_CQ + l * 8, "cq"), (ckvT_d, 4, 8, GV_CKV + l * 4, "ckv"))):
            def loader(c, sl, tb, srcd=srcd, key=key):
                P.dma("sp", xs[sl][:], srcd[c * 128:(c + 1) * 128, tb * TB:(tb + 1) * TB], reads=[key], writes=[("cxs", sl)],
                      key=("cxs", sl))
                return ("cxs", sl)
            for tb in range(2):
                rmsnorm_stats(loader, nch, nch * 128, rstd[gi * 2 + tb], tb, xs, sq)
            for tb in range(2):
                for c in range(nch):
                    sl = c % 2
                    k = loader(c, sl, tb)
                    P.op("dve", lambda e: e.scalar_tensor_tensor(out=actT[:, c_off + c, tb * TB:(tb + 1) * TB], in0=xs[sl][:],
                                                                 scalar=gvec[:, gcol + c:gcol + c + 1], in1=rstd[gi * 2 + tb][:],
                                                                 op0=ALU.mult, op1=ALU.mult),
                         reads=[k, "gvec", ("rstd", id(rstd[gi * 2 + tb]))], writes=[actk[c_off + c]])
        sB = 192.0 ** -0.5
        jobs = []
        wk = ("wf", "w_uq", l)
        wfd = wfull[("w_uq", l)]
        for hh in range(16):
            jobs.append((wk, wfd, hh * 192, 192, [("fm", 0, 128, h_store(qbnT, hh * 128, sB)),
                                                  ("fm", 128, 64, h_rope(qbrT, hh * 64, 1, sB))]))
        run_jobs(actT[:, 0:8, :], actk[0:8], 8, jobs, wbuf)
        jobs = []
        wk = ("wf", "w_ukv", l)
        wfd = wfull[("w_ukv", l)]
        for hh in range(16):
            jobs.append((wk, wfd, hh * 256, 256, [("fm", 0, 128, h_store(gk_in, G_KB + hh * 128, key="gk")),
                                                  ("tm", 128, 128, h_tm(vb_d, hh * 128, key="gk"))]))
        run_jobs(actT[:, 8:12, :], actk[8:12], 4, jobs, wbuf)
        if stage < 1.45:
            P.barrier_all()
            return
        P.collective(lambda e: e.collective_compute("AllGather", ALU.bypass, replica_groups=[list(range(NC))],
                                                    ins=[gk_in.ap().opt()], outs=[gk_out.ap().opt()]),
                     "gk%d" % l, reads=["gk"], writes=["gk_out"])
        P.collective(lambda e: e.collective_compute("AllGather", ALU.bypass, replica_groups=[list(range(NC))],
                                                    ins=[gv_in.ap().opt()], outs=[gv_out.ap().opt()]),
                     "gv%d" % l, reads=["gk"], writes=["gv_out"])
        P.collective(lambda e: e.collective_compute("AllGather", ALU.bypass, replica_groups=[list(range(NC))],
                                                    ins=[gf_in_t.ap().opt()], outs=[gf_out_t.ap().opt()]),
                     "gf%d" % l, reads=["gf_in"], writes=["gf_out"])
        P.barrier_all()


    def vpiece(jk, goff, F, b, c0, n):
        rows = F
        v = gv_out[jk * GV_ROWS + goff:jk * GV_ROWS + goff + rows, :].rearrange("a b -> (a b)").rearrange("(t f) -> t f", f=F)
        return v[b * TB:(b + 1) * TB, c0:c0 + n].rearrange("(g i) f -> i g f", i=128)

    def phase2(l):
        work_reset()
        logfT = sb("logfT", [128, 64, 8], F32)
        cumT = sb("cumT", [128, 64, 8], F32)
        totT = sb("totT", [128, 64, 8], F32)
        pref = sb("pref", [128, 64, 8], F32)
        cref = sb("cref", [128, 8, 8], F32)
        nbias = sb("nbias", [128, 2, 8, 4, 32], F32)
        biasA = [sb("biasA%d" % g, [128, 8 * 128 * (g + 1)], BF16) for g in range(4)]
        mark = K.wptr
        gfv = gf_out.rearrange("(t p) h -> p t h", p=128)
        for jk in range(8):
            P.dma("sp", logfT[:, jk * 8:(jk + 1) * 8, :], gfv[:, jk * 8:(jk + 1) * 8, :], reads=["gf_out"], writes=["logfT"], key="logfT")
        lf = logfT[:].rearrange("p t h -> p (t h)")
        P.op("pe", lambda e: e.matmul(ps[0][:, :], triu[:], lf, start=True, stop=True), reads=["triu", "logfT"], writes=[psk[0]])
        P.op("pe", lambda e: e.matmul(ps[1][:, :], onesf[:], lf, start=True, stop=True), reads=["onesf", "logfT"], writes=[psk[1]])
        P.op("act", lambda e: e.activation(out=cumT[:].rearrange("p t h -> p (t h)"), in_=ps[0][:, :], func=AF.Identity), reads=[psk[0]], writes=["cumT"])
        P.op("act", lambda e: e.activation(out=totT[:].rearrange("p t h -> p (t h)"), in_=ps[1][:, :], func=AF.Identity), reads=[psk[1]], writes=["totT"])
        for b in range(2):
            prev = None
            for g in range(4):
                for jk in range(8):
                    t = jk * 8 + b * 4 + g
                    if prev is None:
                        P.op("dve", lambda e: e.memset(pref[:, t, :], 0.0), writes=["pref"])
                    else:
                        P.op("dve", lambda e: e.tensor_tensor(out=pref[:, t, :], in0=pref[:, prev, :], in1=totT[:, prev, :], op=ALU.add),
                             reads=["pref", "totT"], writes=["pref"])
                    prev = t
        P.op("dve", lambda e: e.tensor_tensor(out=cumT[:], in0=cumT[:], in1=pref[:], op=ALU.add), reads=["cumT", "pref"], writes=["cumT"])
        P.op("dve", lambda e: e.memset(cref[:], 0.0), writes=["cref"])
        for jk in range(8):
            P.op("dve", lambda e: e.scalar_tensor_tensor(out=cref[:], in0=pref[:, jk * 8:(jk + 1) * 8, :], scalar=selt[:, jk:jk + 1],
                                                         in1=cref[:], op0=ALU.mult, op1=ALU.add),
                 reads=["pref", "selt", "cref"], writes=["cref"])
        cum4 = cumT[:].rearrange("p (jk lt) h -> p jk lt h", lt=8)
        for b in range(2):
            for hd in range(8):
                for gq in range(4):
                    P.op("dve", lambda e: e.tensor_scalar(out=nbias[:, b, hd, gq, :].rearrange("p (jk g) -> p jk g", g=4),
                                                          in0=cum4[:, :, b * 4:(b + 1) * 4, hd], scalar1=-1.0,
                                                          scalar2=cref[:, b * 4 + gq, hd:hd + 1], op0=ALU.mult, op1=ALU.add),
                         reads=["cumT", "cref"], writes=["nbias"])

        def indexer(b):
            K.wptr = mark
            iqall = sb("iqall", [64, 32, 128], BF16)
            ikb = sb("ikb", [64, 8, TB], BF16)
            score = sb("score", [128, 8 * TB], F32)
            workt = sb("workt", [128, 8 * TB], F32)
            m8 = sb("m8", [128, 8], F32)
            thr = sb("thr", [128, 1], F32)
            diag = sb("diag", [128, 32, 128], BF16)
            rl = [sb("rl%d" % i, [128, TB], BF16) for i in range(4)]
            iwt = sb("iwt", [128, 32], F32)
            for jk in range(8):
                P.dma("sp", ikb[:, jk, :], gk_out[jk * G_ROWS + G_IK:jk * G_ROWS + G_IK + 64, b * TB:(b + 1) * TB],
                      reads=["gk_out"], writes=["ikb"], key="ikb")
            for gq in range(4):
                Ng = 128 * (gq + 1)
                P.dma("sp", iwt[:], iw_d[b * TB + gq * 128:b * TB + (gq + 1) * 128, :], writes=["iwt"])
                iqv = iqT[:, b * TB + gq * 128:b * TB + (gq + 1) * 128].rearrange("(h d) q -> d h q", d=64)
                for q in range(4):
                    P.dma("sp", iqall[:, q * 8:(q + 1) * 8, :], iqv[:, q * 8:(q + 1) * 8, :], writes=["iqall"], key="iqall")
                for h in range(32):
                    P.op("dve", lambda e: e.tensor_scalar(out=diag[:, h, :], in0=identf[:], scalar1=iwt[:, h:h + 1], scalar2=None,
                                                          op0=ALU.mult), reads=["identf", "iwt"], writes=[("diag", h)])
                for jk in range(8):
                    sbk = 4 + jk % 2
                    pend = None
                    for h in range(32):
                        rb = h % 4
                        P.op("pe", lambda e: e.matmul(ps[rb][:, 0:Ng], iqall[:, h, :], ikb[:, jk, 0:Ng],
                                                      start=True, stop=True), reads=["iqall", "ikb"], writes=[psk[rb]])
                        P.op("act", lambda e: e.activation(out=rl[rb][:, 0:Ng], in_=ps[rb][:, 0:Ng], func=AF.Relu),
                             reads=[psk[rb]], writes=[("rl", rb)])
                        if pend is not None:
                            pend()

                        def acc(h=h, rb=rb):
                            P.op("pe", lambda e: e.matmul(ps[sbk][:, 0:Ng], diag[:, h, :], rl[rb][:, 0:Ng], start=(h == 0), stop=(h == 31)),
                                 reads=[("diag", h), ("rl", rb)], writes=[psk[sbk]])
                        pend = acc
                    pend()
                    P.op("act", lambda e: e.activation(out=score[:, jk * Ng:(jk + 1) * Ng], in_=ps[sbk][:, 0:Ng], func=AF.Identity),
                         reads=[psk[sbk]], writes=["score"])
                    P.op("dve", lambda e: e.tensor_tensor(out=score[:, jk * Ng + gq * 128:(jk + 1) * Ng], in0=score[:, jk * Ng + gq * 128:(jk + 1) * Ng],
                                                          in1=maskQ[:, jk, :], op=ALU.add), reads=["score", "maskQ"], writes=["score"])
                n = 8 * Ng
                for r in range(32):
                    src = score if r == 0 else workt
                    P.op("dve", lambda e: e.max(out=m8[:], in_=src[:, 0:n]), reads=["score", "workt"], writes=["m8"])
                    if r < 31:
                        P.op("dve", lambda e: e.match_replace(out=workt[:, 0:n], in_to_replace=m8[:], in_values=src[:, 0:n], imm_value=-3.0e38),
                             reads=["score", "workt", "m8"], writes=["workt"])
                P.op("dve", lambda e: e.tensor_scalar(out=thr[:], in0=m8[:, 7:8], scalar1=-1.0e29, scalar2=None, op0=ALU.max),
                     reads=["m8"], writes=["thr"])
                P.op("dve", lambda e: e.tensor_scalar(out=biasA[gq][:, 0:n], in0=score[:, 0:n], scalar1=thr[:, 0:1], scalar2=NEGB,
                                                      op0=ALU.is_lt, op1=ALU.mult), reads=["score", "thr"], writes=[("biasA", gq)])

        def attention(b):
            K.wptr = mark
            Kt = [sb("Kt%d" % i, [128, 8, TB], BF16) for i in range(2)]
            Kr = sb("Kr", [64, 8, TB], BF16)
            Vt = [sb("Vt%d" % i, [128, 32, 256], BF16) for i in range(2)]
            Qt = [sb("Qt%d" % i, [128, TB], BF16) for i in range(2)]
            Qr = [sb("Qr%d" % i, [64, TB], BF16) for i in range(2)]
            Pt = [sb("Pt%d" % i, [128, TB], BF16) for i in range(3)]
            rden = sb("rden", [128, TB], F32)
            onum = sb("onum", [128, TB], F32)
            kts = [(jk, gk) for jk in range(8) for gk in range(4)]
            for jk in range(8):
                P.dma("sp", Kr[:, jk, :], gk_out[jk * G_ROWS + G_KR:jk * G_ROWS + G_KR + 64, b * TB:(b + 1) * TB],
                      reads=["gk_out"], writes=["Kr"], key="Kr")
            heads = []
            for hd in range(8):
                heads.append(("A", hd, hd, G_KA, G_VA, 1024, qaT, None))
            for hd in range(16):
                heads.append(("B", hd, 8 + hd, G_KB, G_VB, 2048, qbnT, qbrT))
            for hd in range(8):
                heads.append(("C", hd, 24 + hd, G_KCC, G_VC, 1024, qcT, None))

            def load_head(hi):
                kind, hd, chunk, gko, gvo, F, qd, qrd = heads[hi]
                s = hi % 2
                for jk in range(8):
                    P.dma("sp", Kt[s][:, jk, :], gk_out[jk * G_ROWS + gko + hd * 128:jk * G_ROWS + gko + (hd + 1) * 128, b * TB:(b + 1) * TB],
                          reads=["gk_out"], writes=[("Kt", s)], key=("Kt", s))
                P.dma("sp", Qt[s][:], qd[hd * 128:(hd + 1) * 128, b * TB:(b + 1) * TB], writes=[("Qt", s)], key=("Qt", s))
                if qrd is not None:
                    P.dma("sp", Qr[s][:], qrd[hd * 64:(hd + 1) * 64, b * TB:(b + 1) * TB], writes=[("Qr", s)], key=("Qr", s))
                if hd % 2 == 0:
                    vs = (hi // 2) % 2
                    for jk in range(8):
                        P.dma("sp", Vt[vs][:, jk * 4:(jk + 1) * 4, :], vpiece(jk, gvo, F, b, hd * 128, 256),
                              reads=["gv_out"], writes=[("Vt", vs)], key=("Vt", vs))

            K.pslot = 0

            def do_head(hi):
                kind, hd, chunk, gko, gvo, F, qd, qrd = heads[hi]
                s = hi % 2
                vs = (hi // 2) % 2
                dbk = 2 + hi % 2
                obk = 4 + hi % 2
                mset = 1 if kind == "C" else 0
                vcol = (hd % 2) * 128
                slots = {}

                def qk(idx):
                    jk, gk = kts[idx]
                    nq = 128 * (4 - gk)
                    q0 = gk * 128
                    sbk = idx % 2
                    P.op("pe", lambda e: e.matmul(ps[sbk][:, 0:nq], Kt[s][:, jk, gk * 128:(gk + 1) * 128], Qt[s][:, q0:TB],
                                                  start=True, stop=False), reads=[("Kt", s), ("Qt", s)], writes=[psk[sbk]])
                    if kind == "B":
                        P.op("pe", lambda e: e.matmul(ps[sbk][:, 0:nq], Kr[:, jk, gk * 128:(gk + 1) * 128], Qr[s][:, q0:TB],
                                                      start=False, stop=False), reads=["Kr", ("Qr", s)], writes=[psk[sbk]])
                    if kind == "A":
                        for gq in range(gk, 4):
                            Ng = 128 * (gq + 1)
                            P.op("pe", lambda e: e.matmul(ps[sbk][:, (gq - gk) * 128:(gq - gk + 1) * 128],
                                                          biasA[gq][:, jk * Ng + gk * 128:jk * Ng + (gk + 1) * 128], identb[:],
                                                          start=False, stop=(gq == 3)),
                                 reads=[("biasA", gq), "identb"], writes=[psk[sbk]])
                    else:
                        P.op("pe", lambda e: e.matmul(ps[sbk][:, 0:128], maskB[:, mset, jk, :], identb[:], start=False, stop=True),
                             reads=["maskB", "identb"], writes=[psk[sbk]])

                def rest(idx):
                    jk, gk = kts[idx]
                    nq = 128 * (4 - gk)
                    q0 = gk * 128
                    sbk = idx % 2
                    K.pslot = (K.pslot + 1) % 3
                    pt = Pt[K.pslot]
                    pk = ("Pt", K.pslot)
                    if kind == "C":
                        for gq in range(gk, 4):
                            c0 = (gq - gk) * 128
                            P.op("act", lambda e: e.activation(out=pt[:, c0:c0 + 128], in_=ps[sbk][:, c0:c0 + 128], func=AF.Exp,
                                                               bias=nbias[:, b, hd, gq, jk * 4 + gk:jk * 4 + gk + 1], scale=1.0),
                                 reads=[psk[sbk], "nbias"], writes=[pk])
                    else:
                        P.op("act", lambda e: e.activation(out=pt[:, 0:nq], in_=ps[sbk][:, 0:nq], func=AF.Exp),
                             reads=[psk[sbk]], writes=[pk])
                    P.op("pe", lambda e: e.matmul(ps[dbk][:, q0:TB], onesb[:], pt[:, 0:nq], start=(idx == 0), stop=(idx == 31)),
                         reads=["onesb", pk], writes=[psk[dbk]])
                    P.op("pe", lambda e: e.matmul(ps[obk][:, q0:TB], Vt[vs][:, jk * 4 + gk, vcol:vcol + 128], pt[:, 0:nq],
                                                  start=(idx == 0), stop=(idx == 31)),
                         reads=[("Vt", vs), pk], writes=[psk[obk]])

                qk(0)
                for idx in range(32):
                    if idx + 1 < 32:
                        qk(idx + 1)
                    rest(idx)
                P.op("act", lambda e: e.activation(out=rden[:], in_=ps[dbk][:, :], func=AF.Identity), reads=[psk[dbk]], writes=["rden"])
                P.op("act", lambda e: e.activation(out=onum[:], in_=ps[obk][:, :], func=AF.Identity), reads=[psk[obk]], writes=["onum"])
                P.op("dve", lambda e: e.reciprocal(out=rden[:], in_=rden[:]), reads=["rden"], writes=["rden"])
                P.op("dve", lambda e: e.tensor_tensor(out=actT[:, chunk, b * TB:(b + 1) * TB], in0=onum[:], in1=rden[:], op=ALU.mult),
                     reads=["onum", "rden"], writes=[actk[chunk]])

            load_head(0)
            for hi in range(len(heads)):
                if hi + 1 < len(heads):
                    load_head(hi + 1)
                do_head(hi)

        for b in range(2):
            if stage >= 3:
                indexer(b)
            P.barrier_all()
            if stage >= 4:
                attention(b)
            P.barrier_all()

    def resid_handler(l, gate_k, xsrc, stf, rows_of, key="xT"):
        def h(bk, n, tb, row0):
            K.si += 1
            s = K.si % 4
            sk = ("stf", s)
            P.dma("sp", stf[s][0:n, :], xsrc[row0:row0 + n, tb * TB:(tb + 1) * TB], reads=[key], writes=[sk], key=("xo", s))
            s8 = K.si % 2
            P.op("act", lambda e: e.activation(out=K.rtmp[s8][0:n, :], in_=ps[bk][0:n, :], func=AF.Identity,
                                               scale=mod_ap(l, gate_k, row0 // 128, tb)), reads=[psk[bk], "modT"], writes=[("rtmp", s8)])
            P.op("dve", lambda e: e.tensor_tensor(out=stf[s][0:n, :], in0=stf[s][0:n, :], in1=K.rtmp[s8][0:n, :], op=ALU.add),
                 reads=[("rtmp", s8), sk], writes=[sk])
            P.dma("sp", xT[row0:row0 + n, tb * TB:(tb + 1) * TB], stf[s][0:n, :], reads=[sk], writes=[key], key=("st", key))
        return h

    def phase3(l):
        work_reset()
        K.si = 0
        xsrc = xT_in if l == 0 else xT
        sq = [sb("gsq%d" % i, [128, TB], F32) for i in range(2)]
        rstd = [sb("grstd%d" % i, [128, TB], F32) for i in range(6)]
        wbuf = [sb("wbuf3_%d" % i, [128, KC, 256], BF16) for i in range(3)]
        stf = [sb("stf3_%d" % i, [128, TB], F32) for i in range(4)]
        stb = [sb("stb3_%d" % i, [128, TB], BF16) for i in range(4)]
        K.rtmp = [sb("rtmp3_%d" % i, [128, TB], F32) for i in range(2)]
        for gi, (c0, c1) in enumerate(((0, 8), (8, 24), (24, 32))):
            nch = c1 - c0
            for tb in range(2):
                r = rstd[gi * 2 + tb]
                for c in range(c0, c1):
                    sl = c % 2
                    P.op("act", lambda e: e.activation(out=sq[sl][:], in_=actT[:, c, tb * TB:(tb + 1) * TB], func=AF.Square),
                         reads=[actk[c]], writes=[("sq", sl)])
                    P.op("pe", lambda e: e.matmul(ps[6][:, :], onesf[:], sq[sl][:], start=(c == c0), stop=(c == c1 - 1)),
                         reads=[("sq", sl), "onesf"], writes=[psk[6]])
                P.op("act", lambda e: e.activation(out=r[:], in_=ps[6][:, :], func=AF.Sqrt, bias=K.epst[:, 0:1], scale=1.0 / (nch * 128)),
                     reads=[psk[6], "epst"], writes=[("rstd", id(r))])
                P.op("dve", lambda e: e.reciprocal(out=r[:], in_=r[:]), reads=[("rstd", id(r))], writes=[("rstd", id(r))])
                for c in range(c0, c1):
                    gcol = GV_OUT + l * 32 + c
                    P.op("dve", lambda e: e.scalar_tensor_tensor(out=actT[:, c, tb * TB:(tb + 1) * TB], in0=actT[:, c, tb * TB:(tb + 1) * TB],
                                                                 scalar=gvec[:, gcol:gcol + 1], in1=r[:], op0=ALU.mult, op1=ALU.mult),
                         reads=[actk[c], "gvec", ("rstd", id(r))], writes=[actk[c]])
        rh = resid_handler(l, 2, xsrc, stf, None)
        jobs = []
        wk = ("wf", "w_out", l)
        wfd = wfull[("w_out", l)]
        for j in range(0, D, 256):
            subs = [("fm", m, 128, (lambda bk, n, tb, r0=j + m: rh(bk, n, tb, r0))) for m in (0, 128)]
            jobs.append((wk, wfd, j, 256, subs))
        run_jobs(actT, actk, KC, jobs, wbuf)
        P.barrier_all()
        if stage < 6:
            return
        norm_modulate(l, 1, xT, "xT")
        P.barrier_all()
        work_reset()
        K.si = 0
        wbuf = [sb("wbuf4_%d" % i, [128, KC, 256], BF16) for i in range(3)]
        stf = [sb("stf4_%d" % i, [128, TB], F32) for i in range(4)]
        stb = [sb("stb4_%d" % i, [128, TB], BF16) for i in range(4)]

        def h_up(row0):
            def h(bk, n, tb):
                K.si += 1
                s = K.si % 4
                P.op("act", lambda e: e.activation(out=stf[s][0:n, :], in_=ps[bk][0:n, :], func=AF.Relu), reads=[psk[bk]], writes=[("stf", s)])
                P.op("dve", lambda e: e.tensor_tensor(out=stb[s][0:n, :], in0=stf[s][0:n, :], in1=stf[s][0:n, :], op=ALU.mult),
                     reads=[("stf", s)], writes=[("stb", s)])
                P.dma("sp", uT[row0:row0 + n, tb * TB:(tb + 1) * TB], stb[s][0:n, :], reads=[("stb", s)], writes=["uT"], key=("st", "uT"))
            return h
        jobs = []
        wk = ("wf", "w_up", l)
        wfd = wfull[("w_up", l)]
        for j in range(0, DFF, 256):
            jobs.append((wk, wfd, j, 256, [("fm", m, 128, h_up(j + m)) for m in (0, 128)]))
        run_jobs(actT, actk, KC, jobs, wbuf)
        P.barrier_all()
        if stage < 7:
            return
        work_reset()
        K.si = 0
        wd = [sb("wd%d" % i, [128, 8, TB], BF16) for i in range(3)]
        ub = [sb("ub%d" % i, [128, 8, T], BF16) for i in range(2)]
        stf = [sb("stf5_%d" % i, [128, TB], F32) for i in range(4)]
        K.rtmp = [sb("rtmp5_%d" % i, [128, TB], F32) for i in range(2)]
        rh = resid_handler(l, 5, xT, stf, None)
        wfd = wfull[("w_down", l)]
        wk = ("wf", "w_down", l)
        pieces = [(nb, pc) for nb in range(8) for pc in range(16)]

        def pre(i):
            if i >= len(pieces):
                return
            nb, pc = pieces[i]
            ws, us = i % 3, i % 2
            wv = wfd[pc * 1024:(pc + 1) * 1024, nb * TB:(nb + 1) * TB].rearrange("(kc p) n -> p kc n", p=128)
            uv = uT[pc * 1024:(pc + 1) * 1024, :].rearrange("(kc p) t -> p kc t", p=128)
            P.dma("sp", wd[ws][:], wv, reads=[wk], writes=[("wd", ws)], key=("wd", ws))
            P.dma("sp", ub[us][:], uv, reads=["uT"], writes=[("ub", us)], key=("ub", us))
        pre(0)
        for i, (nb, pc) in enumerate(pieces):
            pre(i + 1)
            ws, us = i % 3, i % 2
            for kc in range(8):
                for m in range(4):
                    for tb in range(2):
                        bk = m * 2 + tb
                        P.op("pe", lambda e: e.matmul(ps[bk][:, :], wd[ws][:, kc, m * 128:(m + 1) * 128], ub[us][:, kc, tb * TB:(tb + 1) * TB],
                                                      start=(pc == 0 and kc == 0), stop=(pc == 15 and kc == 7)),
                             reads=[("wd", ws), ("ub", us)], writes=[psk[bk]])
            if pc == 15:
                for m in range(4):
                    for tb in range(2):
                        rh(m * 2 + tb, 128, tb, nb * TB + m * 128)
        P.barrier_all()

    def final_norm():
        work_reset()
        xs = [sb("fxs%d" % i, [128, TB], F32) for i in range(2)]
        sq = [sb("fsq%d" % i, [128, TB], F32) for i in range(2)]
        rstd = [sb("frstd%d" % i, [128, TB], F32) for i in range(2)]
        ot = [sb("fot%d" % i, [128, TB], F32) for i in range(2)]

        def loader(c, sl, tb):
            P.dma("sp", xs[sl][:], xT[c * 128:(c + 1) * 128, tb * TB:(tb + 1) * TB], reads=["xT"], writes=[("xs", sl)], key=("xs", sl))
            return ("xs", sl)
        for tb in range(2):
            rmsnorm_stats(loader, KC, D, rstd[tb], tb, xs, sq)
        for tb in range(2):
            for c in range(KC):
                sl = c % 2
                k = loader(c, sl, tb)
                P.op("dve", lambda e: e.scalar_tensor_tensor(out=ot[sl][:], in0=xs[sl][:], scalar=gvec[:, GV_FIN + c:GV_FIN + c + 1],
                                                             in1=rstd[tb][:], op0=ALU.mult, op1=ALU.mult),
                     reads=[k, "gvec", ("rstd", id(rstd[tb]))], writes=[("fot", sl)])
                P.dma("sp", outT[c * 128:(c + 1) * 128, tb * TB:(tb + 1) * TB], ot[sl][:], reads=[("fot", sl)], writes=["outT"],
                      key=("st", "outT"))

    def dump(name, src, shape, dt, key):
        d = nc.dram_tensor(name, list(shape), dt, kind="ExternalOutput").ap()
        P.dma("sp", d, src, reads=[key], writes=[name], key=("dbg", name))
        dbg_out[name] = 1

    done = False
    for l in range(NL):
        if stage < 1:
            break
        phase1(l)
        if stage < 3:
            break
        phase2(l)
        if stage < 5:
            break
        phase3(l)
        if stage < 8:
            break
    else:
        done = True
    P.barrier_all()
    if done:
        final_norm()
    if "p1" in dbg:
        dump("dbg_qaT", qaT[:, :], [1024, T], BF16, "q1")
        dump("dbg_iqT", iqT[:, :], [2048, T], BF16, "q1")
        dump("dbg_qbnT", qbnT[:, :], [2048, T], BF16, "q1")
        dump("dbg_qbrT", qbrT[:, :], [1024, T], BF16, "q1")
        dump("dbg_qcT", qcT[:, :], [1024, T], BF16, "q1")
        dump("dbg_iw", iw_d[:, :], [T, 32], F32, "q1")
        dump("dbg_gk", gk_out[0:G_ROWS, :], [G_ROWS, T], BF16, "gk_out")
        dump("dbg_gf", gf_out, [8 * T, 8], F32, "gf_out")
        dump("dbg_gv", gv_out[0:GV_ROWS, :], [GV_ROWS, T], BF16, "gv_out")
    if "wf" in dbg:
        dump("dbg_wuq", wfull[("w_uq", 0)][:, :], [1024, 3072], BF16, ("wf", "w_uq", 0))
        dump("dbg_win", wfull[("w_in", 0)][0:D:64, 0:2048], [64, 2048], BF16, ("wf", "w_in", 0))
    if "act" in dbg:
        dump("dbg_actT", actT[:].rearrange("p k t -> p (k t)"), [128, KC * T], BF16, actk[0])
    if "xT" in dbg:
        dump("dbg_xT", xT[:, :], [D, T], F32, "xT")
    P.barrier_all()
    P.finish(["outT"] + list(dbg_out))
    K.dbg_out = dbg_out
    return nc, K


def _const_tables(j):
    ident = np.eye(128, dtype=np.float32)
    p = np.arange(128)
    R128 = np.zeros((128, 128), np.float32)
    R128[(p + 64) % 128, p] = 1.0
    R64 = np.zeros((128, 128), np.float32)
    part64 = (p // 64) * 64 + ((p % 64) + 32) % 64
    R64[part64, p] = 1.0
    triu = (p[:, None] <= p[None, :]).astype(np.float32)
    theta = 10000.0
    inv128 = np.exp(np.arange(0, 128, 2, dtype=np.float32) * np.float32(-np.log(theta) / 128)).astype(np.float32)
    inv64 = np.exp(np.arange(0, 64, 2, dtype=np.float32) * np.float32(-np.log(theta) / 64)).astype(np.float32)
    rtab = np.zeros((128, 4), np.float32)
    rtab[:, 0] = inv128[p % 64] / np.float32(2 * np.pi)
    rtab[:, 1] = inv64[p % 32] / np.float32(2 * np.pi)
    rtab[:, 2] = np.where(p < 64, -1.0, 1.0)
    rtab[:, 3] = np.where((p % 64) < 32, -1.0, 1.0)
    sel = np.zeros((128, 8), np.float32)
    sel[:, j] = 1.0
    cst = np.concatenate([ident, R128, R64, triu, rtab, sel], axis=1).astype(np.float32)
    q = np.arange(128)[:, None]
    s = np.arange(128)[None, :]
    chunk = (s // 64) <= (q // 64)
    frame = s <= q
    maskB = np.zeros((128, 2, 8, 128), np.float32)
    maskQ = np.zeros((128, 8, 128), np.float32)
    for jk in range(8):
        if jk < j:
            continue
        if jk == j:
            maskB[:, 0, jk, :] = np.where(chunk, 0.0, NEGB)
            maskB[:, 1, jk, :] = np.where(frame, 0.0, NEGB)
            maskQ[:, jk, :] = np.where(chunk, 0.0, -1.0e30)
        else:
            maskB[:, :, jk, :] = NEGB
            maskQ[:, jk, :] = -1.0e30
    return cst, maskB.reshape(128, -1), maskQ.reshape(128, -1)


def prep_inputs(inp, NL=2, stage=99):
    x = np.asarray(inp["x"], np.float32)
    pos = np.asarray(inp["positions"], np.int32)
    c = np.asarray(inp["c"], np.float32)
    cT = np.ascontiguousarray(c.reshape(2, 32, 128).transpose(2, 1, 0)).reshape(128, 64)
    gvec = np.zeros((128, 512), np.float32)

    def fm(v):
        return np.asarray(v, np.float32).reshape(-1, 128).T
    for l in range(NL):
        gvec[:, 0 + l * 32:0 + (l + 1) * 32] = fm(inp["g_attn"][l])
        gvec[:, 64 + l * 32:64 + (l + 1) * 32] = fm(inp["g_mlp"][l])
        gvec[:, 160 + l * 8:160 + (l + 1) * 8] = fm(inp["g_cq"][l])
        gvec[:, 176 + l * 4:176 + (l + 1) * 4] = fm(inp["g_ckv"][l])
        gvec[:, 184 + l * 32:184 + (l + 1) * 32] = fm(np.concatenate([inp["g_out_a"][l], inp["g_out_b"][l], inp["g_out_c"][l]]))
    gvec[:, 128:160] = fm(inp["g_final"])
    bfrow = np.ascontiguousarray(np.broadcast_to(np.asarray(inp["b_f"], np.float32)[:NL].reshape(1, NL * 8), (128, NL * 8)))
    maps = []
    for j in range(NC):
        tiles = []
        ptiles = []
        for b in range(2):
            for g in range(4):
                t0 = (8 * g + j) * 128
                tiles.append(x[b, t0:t0 + 128, :])
                ptiles.append(pos[b, t0:t0 + 128])
        xl = np.concatenate(tiles, axis=0)
        cst, maskB, maskQ = _const_tables(j)
        m = {
            "xT_in": np.ascontiguousarray(xl.T),
            "pos": np.concatenate(ptiles).reshape(1, T).astype(np.int32),
            "cT": cT,
            "wada": np.ascontiguousarray(np.asarray(inp["w_ada"])[:NL, :, j * 3072:(j + 1) * 3072]),
            "bada": np.ascontiguousarray(np.asarray(inp["b_ada"], np.float32)[:NL, j * 3072:(j + 1) * 3072].reshape(NL * 24, 128).T),
            "gvec": gvec, "bfrow": bfrow, "cst": cst, "maskB": maskB, "maskQ": maskQ,
        }
        for n, rows in (("w_in", D), ("w_uq", 1024), ("w_ukv", 512), ("w_out", D), ("w_up", D), ("w_down", DFF)):
            if n not in wnames_for(stage):
                continue
            r = rows // 8
            m[n] = np.ascontiguousarray(np.asarray(inp[n])[:NL, j * r:(j + 1) * r, :])
        maps.append(m)
    return maps


def assemble(results, name="outT"):
    out = np.zeros((2, 4096, D), np.float32)
    for j in range(NC):
        o = np.asarray(results[j][name]).T
        for b in range(2):
            for g in range(4):
                t0 = (8 * g + j) * 128
                lt = b * 4 + g
                out[b, t0:t0 + 128, :] = o[lt * 128:(lt + 1) * 128, :]
    return out


_CACHE = {}


def kernel(**inputs):
    if "nc" not in _CACHE:
        _CACHE["nc"] = build(NL=2)[0]
    maps = prep_inputs(inputs, NL=2)
    res = run_bass_kernel_spmd(_CACHE["nc"], maps, core_ids=list(range(NC)))
    return assemble(res.results)
```

```python
import numpy as np
import concourse.bass as bass
import concourse.mybir as mybir
from concourse.bass_utils import run_bass_kernel_spmd

F32 = mybir.dt.float32
BF16 = mybir.dt.bfloat16
I32 = mybir.dt.int32
AF = mybir.ActivationFunctionType
ALU = mybir.AluOpType

NC = 8
D = 4096
T = 1024
TB = 512
KC = 32
DFF = 16384
INW = 9896
EPS = 1e-6
NEGB = -30000.0
import os
sys_argv_jobs = [int(os.environ.get("MKJOBS", "99"))]
ARENA_BYTES = 206000

G_KA, G_IK, G_KR, G_KB, G_KCC, G_ROWS = 0, 1024, 1088, 1152, 3200, 4224
G_VA, G_VB, G_VC, GV_ROWS = 0, 1024, 3072, 4096


class Prog:
    def __init__(self, nc):
        self.nc = nc
        self.E = {"pe": nc.tensor, "act": nc.scalar, "dve": nc.vector, "pool": nc.gpsimd, "sp": nc.sync}
        self.sem = {}
        self.cnt = {}
        for e in ("pe", "act", "dve", "pool"):
            self.sem[e] = nc.alloc_semaphore("s_" + e)
            self.cnt[e] = 0
        self.known = {e: {} for e in self.E}
        self.lastw = {}
        self.reads = {}
        self.nwaits = 0
        self.nops = 0
        self.noflush = set()
        self.trace = {e: [] for e in self.E}

    def simulate(self):
        val = {s: 0 for s in self.sem}
        pc = {e: 0 for e in self.E}
        prog = True
        while prog:
            prog = False
            for e in self.E:
                tr = self.trace[e]
                while pc[e] < len(tr):
                    kind, s, c, tag = tr[pc[e]]
                    if kind == "wait":
                        if val[s] < c:
                            break
                    else:
                        val[s] += c
                    pc[e] += 1
                    prog = True
        stuck = {e: (pc[e], len(self.trace[e]), self.trace[e][pc[e]] if pc[e] < len(self.trace[e]) else None) for e in self.E}
        return stuck, val

    def _dsem(self, key):
        k = ("dma", key)
        if k not in self.sem:
            self.sem[k] = self.nc.alloc_semaphore("d_%d" % len(self.sem))
            self.cnt[k] = 0
        return k

    def _wait(self, eng, deps):
        need = {}
        for (s, c) in deps:
            if s == eng and eng == "pe":
                continue
            if isinstance(s, tuple):
                c = self.cnt[s]
            if c > need.get(s, 0):
                need[s] = c
        kn = self.known[eng]
        for s, c in need.items():
            if kn.get(s, 0) >= c:
                continue
            self.E[eng].wait_ge(self.sem[s], c)
            self.trace[eng].append(("wait", s, c, None))
            kn[s] = c
            self.nwaits += 1

    def _deps(self, reads, writes):
        deps = []
        for k in reads:
            t = self.lastw.get(k)
            if t is not None:
                deps.append(t)
        for k in writes:
            t = self.lastw.get(k)
            if t is not None:
                deps.append(t)
            deps.extend(self.reads.get(k, ()))
        return deps

    def _commit(self, tok, reads, writes):
        for k in writes:
            self.lastw[k] = tok
            self.reads[k] = []
        for k in reads:
            lst = self.reads.setdefault(k, [])
            lst[:] = [t for t in lst if t[0] != tok[0]]
            lst.append(tok)

    def op(self, eng, fn, reads=(), writes=()):
        self._wait(eng, self._deps(reads, writes))
        ins = fn(self.E[eng])
        self.cnt[eng] += 1
        ins.then_inc(self.sem[eng], 1)
        self.trace[eng].append(("inc", eng, 1, (tuple(reads), tuple(writes))))
        self._commit((eng, self.cnt[eng]), reads, writes)
        self.nops += 1
        return ins

    def dma(self, q, out, in_, reads=(), writes=(), key=None, group=None):
        if group is not None:
            self.gcount = getattr(self, "gcount", 0) + 1
            writes = [(group, "u", self.gcount)]
        self._wait(q, self._deps(reads, writes))
        sk = self._dsem(key if key is not None else (tuple(writes), q))
        ins = self.E[q].dma_start(out=out, in_=in_)
        self.cnt[sk] += 16
        ins.then_inc(self.sem[sk], 16)
        self.trace[q].append(("inc", sk, 16, (tuple(reads), tuple(writes))))
        self._commit((sk, self.cnt[sk]), reads, writes)
        if group is not None:
            self.lastw[group] = (sk, self.cnt[sk])
        self.nops += 1
        return ins

    def collective(self, fn, name, reads=(), writes=()):
        self._wait("pool", self._deps(reads, writes))
        sk = self._dsem(("cc", name))
        ins = fn(self.E["pool"])
        self.cnt[sk] += 1
        ins.then_inc(self.sem[sk])
        self.trace["pool"].append(("inc", sk, 1, (tuple(reads), tuple(writes))))
        self._commit((sk, 1), reads, writes)
        self.noflush.add(sk) if name.startswith("w_") else None
        return ins

    def finish(self, keys, eng="sp"):
        self._wait(eng, [self.lastw[k] for k in keys if k in self.lastw])

    def barrier_all(self):
        allt = [(s, c) for s, c in self.cnt.items() if c > 0 and s not in self.noflush]
        for e in self.E:
            self._wait(e, [t for t in allt if t[0] != e])


class Ctx:
    pass


def wnames_for(stage):
    if stage < 0.5:
        return []
    n = ["w_in", "w_uq", "w_ukv"]
    if stage >= 5:
        n.append("w_out")
    if stage >= 6:
        n.append("w_up")
    if stage >= 7:
        n.append("w_down")
    return n


def _dts(dt):
    return 4 if dt in (F32, I32) else 2


def build(NL=2, stage=99, dbg=()):
    nc = bass.Bass("TRN2", target_bir_lowering=False)
    P = Prog(nc)
    K = Ctx()
    K.nc, K.P = nc, P

    def din(name, shape, dt=F32):
        return nc.dram_tensor(name, list(shape), dt, kind="ExternalInput").ap()

    def dint(name, shape, dt):
        return nc.dram_tensor(name, list(shape), dt)

    xT_in = din("xT_in", [D, T])
    pos_in = din("pos", [1, T], I32)
    cT_in = din("cT", [128, 64])
    wada_in = din("wada", [NL, D, 3072])
    bada_in = din("bada", [128, NL * 24])
    gvec_in = din("gvec", [128, 512])
    bfrow_in = din("bfrow", [128, NL * 8])
    cst_in = din("cst", [128, 4 * 128 + 4 + 8])
    maskB_in = din("maskB", [128, 2 * 8 * 128])
    maskQ_in = din("maskQ", [128, 8 * 128])
    wshape = {"w_in": (D, INW), "w_uq": (1024, 3072), "w_ukv": (512, 4096), "w_out": (D, D),
              "w_up": (D, DFF), "w_down": (DFF, D)}
    wsh = {n: din(n, [NL, wshape[n][0] // 8, wshape[n][1]]) for n in wnames_for(stage)}
    outT = nc.dram_tensor("outT", [D, T], F32, kind="ExternalOutput").ap()
    dbg_out = {}

    xT = dint("xT", [D, T], F32)
    gmod_in = dint("gmod_in", [128, NL * 48], F32)
    gmod_out = dint("gmod_out", [8 * 128, NL * 48], F32)
    wg_in = {(n, l): dint("wgi_%s_%d" % (n, l), [wshape[n][0] // 8, wshape[n][1]], BF16) for n in wsh for l in range(NL)}
    wfull = {(n, l): dint("wf_%s_%d" % (n, l), [wshape[n][0], wshape[n][1]], BF16) for n in wsh for l in range(NL)}
    qaT = dint("qaT", [1024, T], BF16)
    iqT = dint("iqT", [2048, T], BF16)
    iw_d = dint("iw_d", [T, 32], F32)
    cqT_d = dint("cqT_d", [1024, T], F32)
    ckvT_d = dint("ckvT_d", [512, T], F32)
    qbnT = dint("qbnT", [2048, T], BF16)
    qbrT = dint("qbrT", [1024, T], BF16)
    qcT = dint("qcT", [1024, T], BF16)
    gk_in = dint("gk_in", [G_ROWS, T], BF16)
    gk_out = dint("gk_out", [8 * G_ROWS, T], BF16)
    gv_in = dint("gv_in", [GV_ROWS, T], BF16)
    gv_out = dint("gv_out", [8 * GV_ROWS, T], BF16)
    gf_in_t = dint("gf_in", [8, T], F32)
    gf_out_t = dint("gf_out", [64, T], F32)
    gf_in = gf_in_t[:, :].rearrange("a b -> (a b)").rearrange("(t h) -> t h", h=8)
    gf_out = gf_out_t[:, :].rearrange("a b -> (a b)").rearrange("(t h) -> t h", h=8)
    uT = dint("uT", [DFF, T], BF16)

    arena = nc.alloc_sbuf_tensor("arena", [128, ARENA_BYTES // 2], BF16)
    abase = nc.lookup_mloc(arena).addr
    K.pptr = 0
    K.wbase = None
    K.wptr = 0
    K.uid = 0

    def sb(name, shape, dt, work=True):
        nb = _dts(dt)
        for s in shape[1:]:
            nb *= s
        nb = (nb + 63) // 64 * 64
        K.uid += 1
        if work:
            off = K.wbase + K.wptr
            K.wptr += nb
        else:
            assert K.wbase is None
            off = K.pptr
            K.pptr += nb
        assert off + nb <= ARENA_BYTES, (name, off, nb)
        return nc.alloc_sbuf_tensor_at("%s_%d" % (name, K.uid), list(shape), dt, offset=abase + off)

    def work_reset():
        if K.wbase is None:
            K.wbase = K.pptr
        K.wptr = 0

    ps = [nc.alloc_psum_tensor("ps%d" % i, [128, 512], F32) for i in range(8)]
    psk = [("ps", i) for i in range(8)]

    actT = sb("actT", [128, KC, T], BF16, work=False)
    cosT = [sb("cos%d" % v, [128, T], F32, work=False) for v in range(2)]
    sinT = [sb("sin%d" % v, [128, T], F32, work=False) for v in range(2)]
    identf = sb("identf", [128, 128], F32, work=False)
    identb = sb("identb", [128, 128], BF16, work=False)
    Rb = [sb("R%d" % v, [128, 128], BF16, work=False) for v in range(2)]
    onesf = sb("onesf", [128, 128], F32, work=False)
    onesb = sb("onesb", [128, 128], BF16, work=False)
    triu = sb("triu", [128, 128], F32, work=False)
    rtab = sb("rtab", [128, 4], F32, work=False)
    selt = sb("selt", [128, 8], F32, work=False)
    gvec = sb("gvec", [128, 512], F32, work=False)
    bfrow = sb("bfrow", [128, NL * 8], F32, work=False)
    bada = sb("bada", [128, NL * 24], F32, work=False)
    modT = sb("modT", [128, 8, NL * 48], F32, work=False)
    s1T = sb("s1T", [128, NL * 2 * KC * 2], F32, work=False)
    maskB = sb("maskB", [128, 2, 8, 128], BF16, work=False)
    maskQ = sb("maskQ", [128, 8, 128], F32, work=False)
    condT = sb("condT", [128, 64], F32, work=False)
    epst_p = sb("epst_p", [128, 1], F32, work=False)
    work_reset()

    GV_ATTN, GV_MLP, GV_FIN, GV_CQ, GV_CKV, GV_OUT = 0, 64, 128, 160, 176, 184

    def mod_ap(l, k, fc, b=None):
        c = k * 32 + fc
        r, cj = c // 24, c % 24
        col = (l * 24 + cj) * 2
        if b is None:
            return modT[:, r, col:col + 2]
        return modT[:, r, col + b:col + b + 1]

    def s_ap(l, which, fc, b):
        o = ((l * 2 + which) * KC + fc) * 2 + b
        return s1T[:, o:o + 1]

    P.dma("sp", identf[:], cst_in[:, 0:128], writes=["identf"])
    P.dma("pool", identb[:], cst_in[:, 0:128], writes=["identb"])
    P.dma("pool", Rb[0][:], cst_in[:, 128:256], writes=["R0"])
    P.dma("pool", Rb[1][:], cst_in[:, 256:384], writes=["R1"])
    P.dma("sp", triu[:], cst_in[:, 384:512], writes=["triu"])
    P.dma("sp", rtab[:], cst_in[:, 512:516], writes=["rtab"])
    P.dma("sp", selt[:], cst_in[:, 516:524], writes=["selt"])
    P.dma("sp", gvec[:], gvec_in, writes=["gvec"])
    P.dma("sp", bfrow[:], bfrow_in, writes=["bfrow"])
    P.dma("sp", bada[:], bada_in, writes=["bada"])
    P.dma("pool", maskB[:].rearrange("p a b c -> p (a b c)"), maskB_in, writes=["maskB"])
    P.dma("sp", maskQ[:].rearrange("p a b -> p (a b)"), maskQ_in, writes=["maskQ"])
    P.dma("sp", condT[:], cT_in, writes=["condT"])
    P.op("dve", lambda e: e.memset(epst_p[:], EPS), writes=["epst"])
    P.op("dve", lambda e: e.memset(onesf[:], 1.0), writes=["onesf"])
    P.op("dve", lambda e: e.memset(onesb[:], 1.0), writes=["onesb"])
    P.op("act", lambda e: e.activation(out=condT[:], in_=condT[:], func=AF.Silu), reads=["condT"], writes=["condT"])

    def weight_gather(n, l):
        src = wsh[n][l]
        rows, cols = wshape[n][0] // 8, wshape[n][1]
        dst = wg_in[(n, l)]
        step = max(1, (1 << 21) // cols)
        r0 = 0
        while r0 < rows:
            r1 = min(rows, r0 + step)
            P.dma("pool", dst[r0:r1, :], src[r0:r1, :], writes=[("wgi", n, l)], key=("wgi", n, l))
            r0 = r1
        P.collective(lambda e: e.collective_compute("AllGather", ALU.bypass, replica_groups=[list(range(NC))],
                                                    ins=[dst.ap().opt()], outs=[wfull[(n, l)].ap().opt()]),
                     "w_%s_%d" % (n, l), reads=[("wgi", n, l)], writes=[("wf", n, l)])

    wa = [sb("wa%d" % i, [128, KC, 128], F32) for i in range(2)]
    modloc = sb("modloc", [128, NL * 48], F32)
    for l in range(NL):
        for cj in range(24):
            s = (l * 24 + cj) % 2
            wv = wada_in[l][:, cj * 128:(cj + 1) * 128].rearrange("(kc p) n -> p kc n", p=128)
            for q in range(4):
                P.dma("sp", wa[s][:, q * 8:(q + 1) * 8, :], wv[:, q * 8:(q + 1) * 8, :], writes=[("wa", s)], key=("wa", s))
            bk = s
            for kc in range(KC):
                P.op("pe", lambda e: e.matmul(ps[bk][:, 0:2], wa[s][:, kc, :], condT[:, 2 * kc:2 * kc + 2],
                                              start=(kc == 0), stop=(kc == KC - 1)),
                     reads=[("wa", s), "condT"], writes=[psk[bk]])
            col = (l * 24 + cj) * 2
            P.op("act", lambda e: e.activation(out=modloc[:, col:col + 2], in_=ps[bk][:, 0:2], func=AF.Identity,
                                               bias=bada[:, l * 24 + cj:l * 24 + cj + 1], scale=1.0),
                 reads=[psk[bk], "bada"], writes=["modloc"])
    P.dma("sp", gmod_in[:, :], modloc[:], reads=["modloc"], writes=["gmod_in"])
    P.collective(lambda e: e.collective_compute("AllGather", ALU.bypass, replica_groups=[list(range(NC))],
                                                ins=[gmod_in.ap().opt()], outs=[gmod_out.ap().opt()]),
                 "mod", reads=["gmod_in"], writes=["gmod_out"])
    order = []
    for l in range(NL):
        for n in wnames_for(stage):
            order.append((n, l))
    for (n, l) in order:
        weight_gather(n, l)
    P.dma("sp", modT[:], gmod_out[:, :].rearrange("(r p) c -> p r c", p=128), reads=["gmod_out"], writes=["modT"])
    for l in range(NL):
        for which in range(2):
            for fc in range(KC):
                gcol = (GV_ATTN if which == 0 else GV_MLP) + l * 32 + fc
                o = ((l * 2 + which) * KC + fc) * 2
                P.op("dve", lambda e: e.tensor_scalar(out=s1T[:, o:o + 2], in0=mod_ap(l, 1 + 3 * which, fc), scalar1=1.0,
                                                      scalar2=gvec[:, gcol:gcol + 1], op0=ALU.add, op1=ALU.mult),
                     reads=["modT", "gvec"], writes=["s1T"])

    posb = sb("posb", [128, T], I32)
    posf = sb("posf", [128, T], F32)
    tq = sb("tq", [128, T], F32)
    tki = sb("tki", [128, T], I32)
    tkf = sb("tkf", [128, T], F32)
    P.dma("sp", posb[:], pos_in.partition_broadcast(128), writes=["posb"])
    P.op("dve", lambda e: e.tensor_copy(out=posf[:], in_=posb[:]), reads=["posb"], writes=["posf"])
    for v in range(2):
        for (tab, off) in ((sinT[v], 0.0), (cosT[v], 0.25)):
            P.op("dve", lambda e: e.tensor_scalar(out=tq[:], in0=posf[:], scalar1=rtab[:, v:v + 1], scalar2=off,
                                                  op0=ALU.mult, op1=ALU.add), reads=["posf", "rtab"], writes=["tq"])
            P.op("dve", lambda e: e.tensor_copy(out=tki[:], in_=tq[:]), reads=["tq"], writes=["tki"])
            P.op("dve", lambda e: e.tensor_copy(out=tkf[:], in_=tki[:]), reads=["tki"], writes=["tkf"])
            P.op("dve", lambda e: e.tensor_tensor(out=tq[:], in0=tq[:], in1=tkf[:], op=ALU.subtract),
                 reads=["tq", "tkf"], writes=["tq"])
            P.op("act", lambda e: e.activation(out=tab[:], in_=tq[:], func=AF.Sin, scale=6.28318),
                 reads=["tq"], writes=[("rt", v)])
        P.op("dve", lambda e: e.tensor_scalar(out=sinT[v][:], in0=sinT[v][:], scalar1=rtab[:, 2 + v:3 + v], scalar2=None,
                                              op0=ALU.mult), reads=[("rt", v), "rtab"], writes=[("rt", v)])

    if "mod" in dbg:
        d = nc.dram_tensor("dbg_mod", [128, 8 * NL * 48], F32, kind="ExternalOutput").ap()
        P.dma("sp", d, modT[:].rearrange("p a b -> p (a b)"), reads=["modT"], writes=["dbg_mod"])
        dbg_out["dbg_mod"] = 1
        d = nc.dram_tensor("dbg_rope", [128, 4 * T], F32, kind="ExternalOutput").ap()
        for i, tt in enumerate((cosT[0], sinT[0], cosT[1], sinT[1])):
            P.dma("sp", d[:, i * T:(i + 1) * T], tt[:], reads=[("rt", i // 2)], writes=["dbg_rope%d" % i])
        dbg_out["dbg_rope"] = 1

    K.bank = 0

    def nextbank(n=4):
        b = K.bank
        K.bank = (K.bank + 1) % n
        return b

    def rmsnorm_stats(src_chunk_loader, nch, n_feat, rstd, tb, xs, sq):
        for c in range(nch):
            sl = c % 2
            k = src_chunk_loader(c, sl, tb)
            P.op("act", lambda e: e.activation(out=sq[sl][:], in_=xs[sl][:], func=AF.Square), reads=[k], writes=[("sq", sl)])
            P.op("pe", lambda e: e.matmul(ps[6][:, :], onesf[:], sq[sl][:], start=(c == 0), stop=(c == nch - 1)),
                 reads=[("sq", sl), "onesf"], writes=[psk[6]])
        P.op("act", lambda e: e.activation(out=rstd[:], in_=ps[6][:, :], func=AF.Sqrt, bias=K.epst[:, 0:1], scale=1.0 / n_feat),
             reads=[psk[6], "epst"], writes=[("rstd", id(rstd))])
        P.op("dve", lambda e: e.reciprocal(out=rstd[:], in_=rstd[:]), reads=[("rstd", id(rstd))], writes=[("rstd", id(rstd))])

    K.epst = epst_p

    def load_w_tile(wf_key, wfd, col0, ncols, kcn, wt, slot, kc0=0, q="sp"):
        v = wfd[kc0 * 128:(kc0 + kcn) * 128, col0:col0 + ncols].rearrange("(kc p) n -> p kc n", p=128)
        st = 8
        for a in range(0, kcn, st):
            b = min(kcn, a + st)
            P.dma(q, wt[:, a:b, 0:ncols], v[:, a:b, :], reads=[wf_key], writes=[("wb", slot)], key=("wb", slot))

    def run_jobs(X, xkeys, kcn, jobs, wbuf):
        nslot = len(wbuf)

        def pre(i):
            if i < len(jobs):
                wk, wfd, col0, ncols, _ = jobs[i]
                load_w_tile(wk, wfd, col0, ncols, kcn, wbuf[i % nslot], i % nslot)
        pre(0)
        pre(1)
        for i, (wk, wfd, col0, ncols, subs) in enumerate(jobs):
            pre(i + 2)
            wt = wbuf[i % nslot]
            wkey = ("wb", i % nslot)
            for (kind, c0, n, handler) in subs:
                if kind == "fm":
                    for tb in range(2):
                        bk = nextbank()
                        for kc in range(kcn):
                            P.op("pe", lambda e: e.matmul(ps[bk][0:n, :], wt[:, kc, c0:c0 + n], X[:, kc, tb * TB:(tb + 1) * TB],
                                                          start=(kc == 0), stop=(kc == kcn - 1)),
                                 reads=[wkey, xkeys[kc]], writes=[psk[bk]])
                        handler(bk, n, tb)
                else:
                    for tt in range(8):
                        bk = nextbank()
                        for kc in range(kcn):
                            P.op("pe", lambda e: e.matmul(ps[bk][:, 0:n], X[:, kc, tt * 128:(tt + 1) * 128], wt[:, kc, c0:c0 + n],
                                                          start=(kc == 0), stop=(kc == kcn - 1)),
                                 reads=[wkey, xkeys[kc]], writes=[psk[bk]])
                        handler(bk, n, tt)

    actk = [("actT", kc) for kc in range(KC)]

    def norm_modulate(l, which, xsrc, xkey):
        work_reset()
        xs = [sb("xs%d" % i, [128, TB], F32) for i in range(2)]
        sq = [sb("sq%d" % i, [128, TB], F32) for i in range(2)]
        rstd = [sb("rstd%d" % i, [128, TB], F32) for i in range(2)]
        tmp = [sb("ntmp%d" % i, [128, TB], F32) for i in range(2)]

        def loader(c, sl, tb):
            P.dma("sp", xs[sl][:], xsrc[c * 128:(c + 1) * 128, tb * TB:(tb + 1) * TB], reads=[xkey], writes=[("xs", sl)],
                  key=("xs", sl))
            return ("xs", sl)
        for tb in range(2):
            rmsnorm_stats(loader, KC, D, rstd[tb], tb, xs, sq)
        for tb in range(2):
            for c in range(KC):
                sl = c % 2
                k = loader(c, sl, tb)
                P.op("dve", lambda e: e.tensor_tensor(out=tmp[sl][:], in0=xs[sl][:], in1=rstd[tb][:], op=ALU.mult),
                     reads=[k, ("rstd", id(rstd[tb]))], writes=[("ntmp", sl)])
                P.op("act", lambda e: e.activation(out=actT[:, c, tb * TB:(tb + 1) * TB], in_=tmp[sl][:], func=AF.Identity,
                                                   bias=mod_ap(l, 3 * which, c, tb), scale=s_ap(l, which, c, tb)),
                     reads=[("ntmp", sl), "modT", "s1T"], writes=[actk[c]])

    def phase1(l):
        xsrc = xT_in if l == 0 else xT
        norm_modulate(l, 0, xsrc, "xT")
        P.barrier_all()
        if stage < 1.2:
            return
        work_reset()
        wbuf = [sb("wbuf%d" % i, [128, KC, 320], BF16) for i in range(3)]
        stf = [sb("stf%d" % i, [128, TB], F32) for i in range(4)]
        stb = [sb("stb%d" % i, [128, TB], BF16) for i in range(4)]
        xb = [sb("xb%d" % i, [128, TB], BF16) for i in range(2)]
        t1 = [sb("t1%d" % i, [128, TB], F32) for i in range(2)]
        t2 = [sb("t2%d" % i, [128, TB], F32) for i in range(2)]
        K.si = 0

        def slot(n=4):
            K.si += 1
            return K.si % n

        def h_store(dst, row0, scale=1.0, dt=BF16, key="q1"):
            def h(bk, n, tb):
                s = slot()
                P.op("act", lambda e: e.activation(out=stf[s][0:n, :], in_=ps[bk][0:n, :], func=AF.Identity, scale=scale),
                     reads=[psk[bk]], writes=[("stf", s)])
                if dt == BF16:
                    P.op("dve", lambda e: e.tensor_copy(out=stb[s][0:n, :], in_=stf[s][0:n, :]), reads=[("stf", s)], writes=[("stb", s)])
                    st, sk = stb[s], ("stb", s)
                else:
                    st, sk = stf[s], ("stf", s)
                P.dma("sp", dst[row0:row0 + n, tb * TB:(tb + 1) * TB], st[0:n, :], reads=[sk], group=key, key=("st", key))
            return h

        def h_rope(dst, row0, v, scale=1.0, key="q1"):
            def h(bk, n, tb):
                s2 = slot(2)
                s4 = slot()
                rb = 4 + s2
                P.op("act", lambda e: e.activation(out=t1[s2][0:n, :], in_=ps[bk][0:n, :], func=AF.Identity, scale=scale),
                     reads=[psk[bk]], writes=[("t1", s2)])
                P.op("dve", lambda e: e.tensor_copy(out=xb[s2][0:n, :], in_=t1[s2][0:n, :]), reads=[("t1", s2)], writes=[("xb", s2)])
                P.op("pe", lambda e: e.matmul(ps[rb][0:n, :], Rb[v][0:n, 0:n], xb[s2][0:n, :], start=True, stop=True),
                     reads=[("xb", s2), "R%d" % v], writes=[psk[rb]])
                P.op("act", lambda e: e.activation(out=t2[s2][0:n, :], in_=ps[rb][0:n, :], func=AF.Identity),
                     reads=[psk[rb]], writes=[("t2", s2)])
                P.op("dve", lambda e: e.tensor_tensor(out=t1[s2][0:n, :], in0=t1[s2][0:n, :], in1=cosT[v][0:n, tb * TB:(tb + 1) * TB], op=ALU.mult),
                     reads=[("t1", s2), ("rt", v)], writes=[("t1", s2)])
                P.op("dve", lambda e: e.tensor_tensor(out=t2[s2][0:n, :], in0=t2[s2][0:n, :], in1=sinT[v][0:n, tb * TB:(tb + 1) * TB], op=ALU.mult),
                     reads=[("t2", s2), ("rt", v)], writes=[("t2", s2)])
                P.op("dve", lambda e: e.tensor_tensor(out=stb[s4][0:n, :], in0=t1[s2][0:n, :], in1=t2[s2][0:n, :], op=ALU.add),
                     reads=[("t1", s2), ("t2", s2)], writes=[("stb", s4)])
                P.dma("sp", dst[row0:row0 + n, tb * TB:(tb + 1) * TB], stb[s4][0:n, :], reads=[("stb", s4)], group=key,
                      key=("st", key))
            return h

        def h_tm(dst, col0, dt=BF16, key="q1"):
            def h(bk, n, tt):
                s = slot()
                P.op("act", lambda e: e.activation(out=stf[s][:, 0:n], in_=ps[bk][:, 0:n], func=AF.Identity), reads=[psk[bk]], writes=[("stf", s)])
                if dt == BF16:
                    P.op("dve", lambda e: e.tensor_copy(out=stb[s][:, 0:n], in_=stf[s][:, 0:n]), reads=[("stf", s)], writes=[("stb", s)])
                    st, sk = stb[s], ("stb", s)
                else:
                    st, sk = stf[s], ("stf", s)
                P.dma("sp", dst[tt * 128:(tt + 1) * 128, col0:col0 + n], st[:, 0:n], reads=[sk], group=key, key=("st", key))
            return h

        def h_fl(bk, n, tt):
            s = slot()
            st = stf[s]
            sk = ("stf", s)
            P.op("act", lambda e: e.activation(out=st[:, 0:8], in_=ps[bk][:, 0:8], func=AF.Identity), reads=[psk[bk]], writes=[sk])
            P.op("dve", lambda e: e.tensor_tensor(out=st[:, 0:8], in0=st[:, 0:8], in1=bfrow[:, l * 8:(l + 1) * 8], op=ALU.add),
                 reads=[sk, "bfrow"], writes=[sk])
            P.op("act", lambda e: e.activation(out=st[:, 0:8], in_=st[:, 0:8], func=AF.Exp, scale=-1.0), reads=[sk], writes=[sk])
            P.op("act", lambda e: e.activation(out=st[:, 0:8], in_=st[:, 0:8], func=AF.Ln, bias=1.0, scale=1.0), reads=[sk], writes=[sk])
            P.op("dve", lambda e: e.tensor_scalar(out=st[:, 0:8], in0=st[:, 0:8], scalar1=-1.0, scalar2=None, op0=ALU.mult),
                 reads=[sk], writes=[sk])
            P.dma("sp", gf_in[tt * 128:(tt + 1) * 128, :], st[:, 0:8], reads=[sk], group="gf_in", key=("st", "gf"))

        va_d = gv_in[G_VA:G_VA + 1024, :].rearrange("a b -> (a b)").rearrange("(t f) -> t f", f=1024)
        vb_d = gv_in[G_VB:G_VB + 2048, :].rearrange("a b -> (a b)").rearrange("(t f) -> t f", f=2048)
        vc_d = gv_in[G_VC:G_VC + 1024, :].rearrange("a b -> (a b)").rearrange("(t f) -> t f", f=1024)
        wk = ("wf", "w_in", l)
        wfd = wfull[("w_in", l)]
        sA = 128.0 ** -0.5
        jobs = []

        def fm_jobs(c0, width, mk):
            for j in range(0, width, 256):
                subs = []
                for m in range(0, 256, 128):
                    subs.append(("fm", m, 128, mk(j + m)))
                jobs.append((wk, wfd, c0 + j, 256, subs))

        def tm_jobs(c0, width, dst):
            for j in range(0, width, 256):
                jobs.append((wk, wfd, c0 + j, 256, [("tm", 0, 256, h_tm(dst, j, key="gk"))]))
        fm_jobs(5216, 1024, lambda r: h_store(cqT_d, r, dt=F32, key="cq"))
        fm_jobs(6240, 512, lambda r: h_store(ckvT_d, r, dt=F32, key="ckv"))
        fm_jobs(0, 1024, lambda r: h_rope(qaT, r, 0, sA))
        fm_jobs(1024, 1024, lambda r: h_rope(gk_in, G_KA + r, 0, key="gk"))
        tm_jobs(2048, 1024, va_d)
        fm_jobs(3072, 2048, lambda r: h_rope(iqT, r, 1))
        jobs.append((wk, wfd, 5120, 96, [("fm", 0, 64, h_rope(gk_in, G_IK, 1, key="gk")),
                                         ("tm", 64, 32, h_tm(iw_d, 0, dt=F32))]))
        jobs.append((wk, wfd, 6752, 64, [("fm", 0, 64, h_rope(gk_in, G_KR, 1, key="gk"))]))
        fm_jobs(6816, 1024, lambda r: h_store(qcT, r, sA))
        fm_jobs(7840, 1024, lambda r: h_store(gk_in, G_KCC + r, key="gk"))
        tm_jobs(8864, 768, vc_d)
        jobs.append((wk, wfd, 8864 + 768, 264, [("tm", 0, 256, h_tm(vc_d, 768, key="gk")), ("tm", 256, 8, h_fl)]))
        if stage < 1.3:
            jobs = jobs[int(os.environ.get("MKSKIP", "0")):int(sys_argv_jobs[0])]
        run_jobs(actT, actk, KC, jobs, wbuf)
        if stage < 1.4:
            P.barrier_all()
            return

        xs = [sb("cxs%d" % i, [128, TB], F32) for i in range(2)]
        sq = [sb("csq%d" % i, [128, TB], F32) for i in range(2)]
        rstd = [sb("crstd%d" % i, [128, TB], F32) for i in range(4)]
        for gi, (srcd, nch, c_off, gcol, key) in enumerate(((cqT_d, 8, 0, GV_CQ + l * 8, "cq"), (ckvT_d, 4, 8, GV_CKV + l * 4, "ckv"))):
            def loader(c, sl, tb, srcd=srcd, key=key):
                P.dma("sp", xs[sl][:], srcd[c * 128:(c + 1) * 128, tb * TB:(tb + 1) * TB], reads=[key], writes=[("cxs", sl)],
                      key=("cxs", sl))
                return ("cxs", sl)
            for tb in range(2):
                rmsnorm_stats(loader, nch, nch * 128, rstd[gi * 2 + tb], tb, xs, sq)
            for tb in range(2):
                for c in range(nch):
                    sl = c % 2
                    k = loader(c, sl, tb)
                    P.op("dve", lambda e: e.scalar_tensor_tensor(out=actT[:, c_off + c, tb * TB:(tb + 1) * TB], in0=xs[sl][:],
                                                                 scalar=gvec[:, gcol + c:gcol + c + 1], in1=rstd[gi * 2 + tb][:],
                                                                 op0=ALU.mult, op1=ALU.mult),
                         reads=[k, "gvec", ("rstd", id(rstd[gi * 2 + tb]))], writes=[actk[c_off + c]])
        sB = 192.0 ** -0.5
        jobs = []
        wk = ("wf", "w_uq", l)
        wfd = wfull[("w_uq", l)]
        for hh in range(16):
            jobs.append((wk, wfd, hh * 192, 192, [("fm", 0, 128, h_store(qbnT, hh * 128, sB)),
                                                  ("fm", 128, 64, h_rope(qbrT, hh * 64, 1, sB))]))
        run_jobs(actT[:, 0:8, :], actk[0:8], 8, jobs, wbuf)
        jobs = []
        wk = ("wf", "w_ukv", l)
        wfd = wfull[("w_ukv", l)]
        for hh in range(16):
            jobs.append((wk, wfd, hh * 256, 256, [("fm", 0, 128, h_store(gk_in, G_KB + hh * 128, key="gk")),
                                                  ("tm", 128, 128, h_tm(vb_d, hh * 128, key="gk"))]))
        run_jobs(actT[:, 8:12, :], actk[8:12], 4, jobs, wbuf)
        if stage < 1.45:
            P.barrier_all()
            return
        P.collective(lambda e: e.collective_compute("AllGather", ALU.bypass, replica_groups=[list(range(NC))],
                                                    ins=[gk_in.ap().opt()], outs=[gk_out.ap().opt()]),
                     "gk%d" % l, reads=["gk"], writes=["gk_out"])
        P.collective(lambda e: e.collective_compute("AllGather", ALU.bypass, replica_groups=[list(range(NC))],
                                                    ins=[gv_in.ap().opt()], outs=[gv_out.ap().opt()]),
                     "gv%d" % l, reads=["gk"], writes=["gv_out"])
        P.collective(lambda e: e.collective_compute("AllGather", ALU.bypass, replica_groups=[list(range(NC))],
                                                    ins=[gf_in_t.ap().opt()], outs=[gf_out_t.ap().opt()]),
                     "gf%d" % l, reads=["gf_in"], writes=["gf_out"])
        P.barrier_all()


    def vpiece(jk, goff, F, b, c0, n):
        rows = F
        v = gv_out[jk * GV_ROWS + goff:jk * GV_ROWS + goff + rows, :].rearrange("a b -> (a b)").rearrange("(t f) -> t f", f=F)
        return v[b * TB:(b + 1) * TB, c0:c0 + n].rearrange("(g i) f -> i g f", i=128)

    gkr = gk_out[:, :].rearrange("(r g) t -> g r t", g=G_ROWS)

    def vpiece4(goff, F, b, c0, n, r0, r1):
        v = gv_out[:, :].rearrange("(r g) t -> r (g t)", r=8)[r0:r1, goff * 1024:(goff + F) * 1024]
        v = v.rearrange("r (t f) -> r t f", f=F)[:, b * TB:(b + 1) * TB, c0:c0 + n]
        return v.rearrange("r (g i) f -> i r g f", i=128)

    def phase2(l):
        work_reset()
        logfT = sb("logfT", [128, 64, 8], F32)
        cumT = sb("cumT", [128, 64, 8], F32)
        totT = sb("totT", [128, 64, 8], F32)
        pref = sb("pref", [128, 64, 8], F32)
        cref = sb("cref", [128, 8, 8], F32)
        nbias = sb("nbias", [128, 2, 8, 4, 32], F32)
        biasA = [sb("biasA%d" % g, [128, 8 * 128 * (g + 1)], BF16) for g in range(4)]
        mark = K.wptr
        gfv = gf_out.rearrange("(t p) h -> p t h", p=128)
        for jk in range(8):
            P.dma("sp", logfT[:, jk * 8:(jk + 1) * 8, :], gfv[:, jk * 8:(jk + 1) * 8, :], reads=["gf_out"], writes=["logfT"], key="logfT")
        lf = logfT[:].rearrange("p t h -> p (t h)")
        P.op("pe", lambda e: e.matmul(ps[0][:, :], triu[:], lf, start=True, stop=True), reads=["triu", "logfT"], writes=[psk[0]])
        P.op("pe", lambda e: e.matmul(ps[1][:, :], onesf[:], lf, start=True, stop=True), reads=["onesf", "logfT"], writes=[psk[1]])
        P.op("act", lambda e: e.activation(out=cumT[:].rearrange("p t h -> p (t h)"), in_=ps[0][:, :], func=AF.Identity), reads=[psk[0]], writes=["cumT"])
        P.op("act", lambda e: e.activation(out=totT[:].rearrange("p t h -> p (t h)"), in_=ps[1][:, :], func=AF.Identity), reads=[psk[1]], writes=["totT"])
        for b in range(2):
            prev = None
            for g in range(4):
                for jk in range(8):
                    t = jk * 8 + b * 4 + g
                    if prev is None:
                        P.op("dve", lambda e: e.memset(pref[:, t, :], 0.0), writes=["pref"])
                    else:
                        P.op("dve", lambda e: e.tensor_tensor(out=pref[:, t, :], in0=pref[:, prev, :], in1=totT[:, prev, :], op=ALU.add),
                             reads=["pref", "totT"], writes=["pref"])
                    prev = t
        P.op("dve", lambda e: e.tensor_tensor(out=cumT[:], in0=cumT[:], in1=pref[:], op=ALU.add), reads=["cumT", "pref"], writes=["cumT"])
        P.op("dve", lambda e: e.memset(cref[:], 0.0), writes=["cref"])
        for jk in range(8):
            P.op("dve", lambda e: e.scalar_tensor_tensor(out=cref[:], in0=pref[:, jk * 8:(jk + 1) * 8, :], scalar=selt[:, jk:jk + 1],
                                                         in1=cref[:], op0=ALU.mult, op1=ALU.add),
                 reads=["pref", "selt", "cref"], writes=["cref"])
        cum4 = cumT[:].rearrange("p (jk lt) h -> p jk lt h", lt=8)
        for b in range(2):
            for hd in range(8):
                for gq in range(4):
                    P.op("dve", lambda e: e.tensor_scalar(out=nbias[:, b, hd, gq, :].rearrange("p (jk g) -> p jk g", g=4),
                                                          in0=cum4[:, :, b * 4:(b + 1) * 4, hd], scalar1=-1.0,
                                                          scalar2=cref[:, b * 4 + gq, hd:hd + 1], op0=ALU.mult, op1=ALU.add),
                         reads=["cumT", "cref"], writes=["nbias"])

        def indexer(b):
            K.wptr = mark
            iqall = sb("iqall", [64, 32, 128], BF16)
            ikb = sb("ikb", [64, 8, TB], BF16)
            score = sb("score", [128, 8 * TB], F32)
            workt = sb("workt", [128, 8 * TB], F32)
            m8 = sb("m8", [128, 8], F32)
            thr = sb("thr", [128, 1], F32)
            diag = sb("diag", [128, 32, 128], BF16)
            rl = [sb("rl%d" % i, [128, TB], BF16) for i in range(4)]
            iwt = sb("iwt", [128, 32], F32)
            P.dma("sp", ikb[:], gkr[G_IK:G_IK + 64, :, b * TB:(b + 1) * TB], reads=["gk_out"], writes=["ikb"], key="ikb")
            for gq in range(4):
                Ng = 128 * (gq + 1)
                P.dma("sp", iwt[:], iw_d[b * TB + gq * 128:b * TB + (gq + 1) * 128, :], writes=["iwt"])
                iqv = iqT[:, b * TB + gq * 128:b * TB + (gq + 1) * 128].rearrange("(h d) q -> d h q", d=64)
                for q in range(4):
                    P.dma("sp", iqall[:, q * 8:(q + 1) * 8, :], iqv[:, q * 8:(q + 1) * 8, :], writes=["iqall"], key="iqall")
                for h in range(32):
                    P.op("dve", lambda e: e.tensor_scalar(out=diag[:, h, :], in0=identf[:], scalar1=iwt[:, h:h + 1], scalar2=None,
                                                          op0=ALU.mult), reads=["identf", "iwt"], writes=[("diag", h)])
                for jk in range(8):
                    sbk = 4 + jk % 2
                    pend = None
                    for h in range(32):
                        rb = h % 4
                        P.op("pe", lambda e: e.matmul(ps[rb][:, 0:Ng], iqall[:, h, :], ikb[:, jk, 0:Ng],
                                                      start=True, stop=True), reads=["iqall", "ikb"], writes=[psk[rb]])
                        P.op("act", lambda e: e.activation(out=rl[rb][:, 0:Ng], in_=ps[rb][:, 0:Ng], func=AF.Relu),
                             reads=[psk[rb]], writes=[("rl", rb)])
                        if pend is not None:
                            pend()

                        def acc(h=h, rb=rb):
                            P.op("pe", lambda e: e.matmul(ps[sbk][:, 0:Ng], diag[:, h, :], rl[rb][:, 0:Ng], start=(h == 0), stop=(h == 31)),
                                 reads=[("diag", h), ("rl", rb)], writes=[psk[sbk]])
                        pend = acc
                    pend()
                    P.op("act", lambda e: e.activation(out=score[:, jk * Ng:(jk + 1) * Ng], in_=ps[sbk][:, 0:Ng], func=AF.Identity),
                         reads=[psk[sbk]], writes=["score"])
                    P.op("dve", lambda e: e.tensor_tensor(out=score[:, jk * Ng + gq * 128:(jk + 1) * Ng], in0=score[:, jk * Ng + gq * 128:(jk + 1) * Ng],
                                                          in1=maskQ[:, jk, :], op=ALU.add), reads=["score", "maskQ"], writes=["score"])
                n = 8 * Ng
                for r in range(32):
                    src = score if r == 0 else workt
                    P.op("dve", lambda e: e.max(out=m8[:], in_=src[:, 0:n]), reads=["score", "workt"], writes=["m8"])
                    if r < 31:
                        P.op("dve", lambda e: e.match_replace(out=workt[:, 0:n], in_to_replace=m8[:], in_values=src[:, 0:n], imm_value=-3.0e38),
                             reads=["score", "workt", "m8"], writes=["workt"])
                P.op("dve", lambda e: e.tensor_scalar(out=thr[:], in0=m8[:, 7:8], scalar1=-1.0e29, scalar2=None, op0=ALU.max),
                     reads=["m8"], writes=["thr"])
                P.op("dve", lambda e: e.tensor_scalar(out=biasA[gq][:, 0:n], in0=score[:, 0:n], scalar1=thr[:, 0:1], scalar2=NEGB,
                                                      op0=ALU.is_lt, op1=ALU.mult), reads=["score", "thr"], writes=[("biasA", gq)])

        def attention(b):
            K.wptr = mark
            Kt = [sb("Kt%d" % i, [128, 8, TB], BF16) for i in range(2)]
            Kr = sb("Kr", [64, 8, TB], BF16)
            Vt = [sb("Vt%d" % i, [128, 32, 256], BF16) for i in range(2)]
            Qt = [sb("Qt%d" % i, [128, TB], BF16) for i in range(2)]
            Qr = [sb("Qr%d" % i, [64, TB], BF16) for i in range(2)]
            Pt = [sb("Pt%d" % i, [128, TB], BF16) for i in range(3)]
            rden = sb("rden", [128, TB], F32)
            onum = sb("onum", [128, TB], F32)
            kts = [(jk, gk) for jk in range(8) for gk in range(4)]
            P.dma("sp", Kr[:], gkr[G_KR:G_KR + 64, :, b * TB:(b + 1) * TB], reads=["gk_out"], writes=["Kr"], key="Kr")
            heads = []
            for hd in range(8):
                heads.append(("A", hd, hd, G_KA, G_VA, 1024, qaT, None))
            for hd in range(16):
                heads.append(("B", hd, 8 + hd, G_KB, G_VB, 2048, qbnT, qbrT))
            for hd in range(8):
                heads.append(("C", hd, 24 + hd, G_KCC, G_VC, 1024, qcT, None))

            def load_head(hi):
                kind, hd, chunk, gko, gvo, F, qd, qrd = heads[hi]
                s = hi % 2
                P.dma("sp", Kt[s][:], gkr[gko + hd * 128:gko + (hd + 1) * 128, :, b * TB:(b + 1) * TB],
                      reads=["gk_out"], writes=[("Kt", s)], key=("Kt", s))
                P.dma("sp", Qt[s][:], qd[hd * 128:(hd + 1) * 128, b * TB:(b + 1) * TB], writes=[("Qt", s)], key=("Qt", s))
                if qrd is not None:
                    P.dma("sp", Qr[s][:], qrd[hd * 64:(hd + 1) * 64, b * TB:(b + 1) * TB], writes=[("Qr", s)], key=("Qr", s))
                if hd % 2 == 0:
                    vs = (hi // 2) % 2
                    v4 = vpiece4(gvo, F, b, hd * 128, 256, 0, 8)
                    d4 = Vt[vs][:].rearrange("i (r g) f -> i r g f", g=4)
                    for g in range(4):
                        P.dma("sp", d4[:, :, g, :], v4[:, :, g, :], reads=["gv_out"], writes=[("Vt", vs)], key=("Vt", vs))

            K.pslot = 0

            def do_head(hi):
                kind, hd, chunk, gko, gvo, F, qd, qrd = heads[hi]
                s = hi % 2
                vs = (hi // 2) % 2
                dbk = 2 + hi % 2
                obk = 4 + hi % 2
                mset = 1 if kind == "C" else 0
                vcol = (hd % 2) * 128
                slots = {}

                def qk(idx):
                    jk, gk = kts[idx]
                    nq = 128 * (4 - gk)
                    q0 = gk * 128
                    sbk = idx % 2
                    P.op("pe", lambda e: e.matmul(ps[sbk][:, 0:nq], Kt[s][:, jk, gk * 128:(gk + 1) * 128], Qt[s][:, q0:TB],
                                                  start=True, stop=False), reads=[("Kt", s), ("Qt", s)], writes=[psk[sbk]])
                    if kind == "B":
                        P.op("pe", lambda e: e.matmul(ps[sbk][:, 0:nq], Kr[:, jk, gk * 128:(gk + 1) * 128], Qr[s][:, q0:TB],
                                                      start=False, stop=False), reads=["Kr", ("Qr", s)], writes=[psk[sbk]])
                    if kind == "A":
                        for gq in range(gk, 4):
                            Ng = 128 * (gq + 1)
                            P.op("pe", lambda e: e.matmul(ps[sbk][:, (gq - gk) * 128:(gq - gk + 1) * 128],
                                                          biasA[gq][:, jk * Ng + gk * 128:jk * Ng + (gk + 1) * 128], identb[:],
                                                          start=False, stop=(gq == 3)),
                                 reads=[("biasA", gq), "identb"], writes=[psk[sbk]])
                    else:
                        P.op("pe", lambda e: e.matmul(ps[sbk][:, 0:128], maskB[:, mset, jk, :], identb[:], start=False, stop=True),
                             reads=["maskB", "identb"], writes=[psk[sbk]])

                def rest(idx):
                    jk, gk = kts[idx]
                    nq = 128 * (4 - gk)
                    q0 = gk * 128
                    sbk = idx % 2
                    K.pslot = (K.pslot + 1) % 3
                    pt = Pt[K.pslot]
                    pk = ("Pt", K.pslot)
                    if kind == "C":
                        for gq in range(gk, 4):
                            c0 = (gq - gk) * 128
                            P.op("act", lambda e: e.activation(out=pt[:, c0:c0 + 128], in_=ps[sbk][:, c0:c0 + 128], func=AF.Exp,
                                                               bias=nbias[:, b, hd, gq, jk * 4 + gk:jk * 4 + gk + 1], scale=1.0),
                                 reads=[psk[sbk], "nbias"], writes=[pk])
                    else:
                        P.op("act", lambda e: e.activation(out=pt[:, 0:nq], in_=ps[sbk][:, 0:nq], func=AF.Exp),
                             reads=[psk[sbk]], writes=[pk])
                    P.op("pe", lambda e: e.matmul(ps[dbk][:, q0:TB], onesb[:], pt[:, 0:nq], start=(idx == 0), stop=(idx == 31)),
                         reads=["onesb", pk], writes=[psk[dbk]])
                    P.op("pe", lambda e: e.matmul(ps[obk][:, q0:TB], Vt[vs][:, jk * 4 + gk, vcol:vcol + 128], pt[:, 0:nq],
                                                  start=(idx == 0), stop=(idx == 31)),
                         reads=[("Vt", vs), pk], writes=[psk[obk]])

                qk(0)
                for idx in range(32):
                    if idx + 1 < 32:
                        qk(idx + 1)
                    rest(idx)
                P.op("act", lambda e: e.activation(out=rden[:], in_=ps[dbk][:, :], func=AF.Identity), reads=[psk[dbk]], writes=["rden"])
                P.op("act", lambda e: e.activation(out=onum[:], in_=ps[obk][:, :], func=AF.Identity), reads=[psk[obk]], writes=["onum"])
                P.op("dve", lambda e: e.reciprocal(out=rden[:], in_=rden[:]), reads=["rden"], writes=["rden"])
                P.op("dve", lambda e: e.tensor_tensor(out=actT[:, chunk, b * TB:(b + 1) * TB], in0=onum[:], in1=rden[:], op=ALU.mult),
                     reads=["onum", "rden"], writes=[actk[chunk]])

            load_head(0)
            for hi in range(len(heads)):
                if hi + 1 < len(heads):
                    load_head(hi + 1)
                do_head(hi)

        for b in range(2):
            if stage >= 3:
                indexer(b)
            P.barrier_all()
            if stage >= 4:
                attention(b)
            P.barrier_all()

    def resid_handler(l, gate_k, xsrc, stf, rows_of, key="xT"):
        def h(bk, n, tb, row0):
            K.si += 1
            s = K.si % 4
            sk = ("stf", s)
            P.dma("sp", stf[s][0:n, :], xsrc[row0:row0 + n, tb * TB:(tb + 1) * TB], writes=[sk], key=("xo", s))
            s8 = K.si % 2
            P.op("act", lambda e: e.activation(out=K.rtmp[s8][0:n, :], in_=ps[bk][0:n, :], func=AF.Identity,
                                               scale=mod_ap(l, gate_k, row0 // 128, tb)), reads=[psk[bk], "modT"], writes=[("rtmp", s8)])
            P.op("dve", lambda e: e.tensor_tensor(out=stf[s][0:n, :], in0=stf[s][0:n, :], in1=K.rtmp[s8][0:n, :], op=ALU.add),
                 reads=[("rtmp", s8), sk], writes=[sk])
            P.dma("sp", xT[row0:row0 + n, tb * TB:(tb + 1) * TB], stf[s][0:n, :], reads=[sk], group=key, key=("st", key))
        return h

    def phase3(l):
        work_reset()
        K.si = 0
        xsrc = xT_in if l == 0 else xT
        sq = [sb("gsq%d" % i, [128, TB], F32) for i in range(2)]
        rstd = [sb("grstd%d" % i, [128, TB], F32) for i in range(6)]
        wbuf = [sb("wbuf3_%d" % i, [128, KC, 256], BF16) for i in range(3)]
        stf = [sb("stf3_%d" % i, [128, TB], F32) for i in range(4)]
        stb = [sb("stb3_%d" % i, [128, TB], BF16) for i in range(4)]
        K.rtmp = [sb("rtmp3_%d" % i, [128, TB], F32) for i in range(2)]
        for gi, (c0, c1) in enumerate(((0, 8), (8, 24), (24, 32))):
            nch = c1 - c0
            for tb in range(2):
                r = rstd[gi * 2 + tb]
                for c in range(c0, c1):
                    sl = c % 2
                    P.op("act", lambda e: e.activation(out=sq[sl][:], in_=actT[:, c, tb * TB:(tb + 1) * TB], func=AF.Square),
                         reads=[actk[c]], writes=[("sq", sl)])
                    P.op("pe", lambda e: e.matmul(ps[6][:, :], onesf[:], sq[sl][:], start=(c == c0), stop=(c == c1 - 1)),
                         reads=[("sq", sl), "onesf"], writes=[psk[6]])
                P.op("act", lambda e: e.activation(out=r[:], in_=ps[6][:, :], func=AF.Sqrt, bias=K.epst[:, 0:1], scale=1.0 / (nch * 128)),
                     reads=[psk[6], "epst"], writes=[("rstd", id(r))])
                P.op("dve", lambda e: e.reciprocal(out=r[:], in_=r[:]), reads=[("rstd", id(r))], writes=[("rstd", id(r))])
                for c in range(c0, c1):
                    gcol = GV_OUT + l * 32 + c
                    P.op("dve", lambda e: e.scalar_tensor_tensor(out=actT[:, c, tb * TB:(tb + 1) * TB], in0=actT[:, c, tb * TB:(tb + 1) * TB],
                                                                 scalar=gvec[:, gcol:gcol + 1], in1=r[:], op0=ALU.mult, op1=ALU.mult),
                         reads=[actk[c], "gvec", ("rstd", id(r))], writes=[actk[c]])
        rh = resid_handler(l, 2, xsrc, stf, None)
        jobs = []
        wk = ("wf", "w_out", l)
        wfd = wfull[("w_out", l)]
        for j in range(0, D, 256):
            subs = [("fm", m, 128, (lambda bk, n, tb, r0=j + m: rh(bk, n, tb, r0))) for m in (0, 128)]
            jobs.append((wk, wfd, j, 256, subs))
        run_jobs(actT, actk, KC, jobs, wbuf)
        P.barrier_all()
        if stage < 6:
            return
        norm_modulate(l, 1, xT, "xT")
        P.barrier_all()
        work_reset()
        K.si = 0
        wbuf = [sb("wbuf4_%d" % i, [128, KC, 256], BF16) for i in range(3)]
        stf = [sb("stf4_%d" % i, [128, TB], F32) for i in range(4)]
        stb = [sb("stb4_%d" % i, [128, TB], BF16) for i in range(4)]

        def h_up(row0):
            def h(bk, n, tb):
                K.si += 1
                s = K.si % 4
                P.op("act", lambda e: e.activation(out=stf[s][0:n, :], in_=ps[bk][0:n, :], func=AF.Relu), reads=[psk[bk]], writes=[("stf", s)])
                P.op("dve", lambda e: e.tensor_tensor(out=stb[s][0:n, :], in0=stf[s][0:n, :], in1=stf[s][0:n, :], op=ALU.mult),
                     reads=[("stf", s)], writes=[("stb", s)])
                P.dma("sp", uT[row0:row0 + n, tb * TB:(tb + 1) * TB], stb[s][0:n, :], reads=[("stb", s)], group="uT", key=("st", "uT"))
            return h
        jobs = []
        wk = ("wf", "w_up", l)
        wfd = wfull[("w_up", l)]
        for j in range(0, DFF, 256):
            jobs.append((wk, wfd, j, 256, [("fm", m, 128, h_up(j + m)) for m in (0, 128)]))
        run_jobs(actT, actk, KC, jobs, wbuf)
        P.barrier_all()
        if stage < 7:
            return
        work_reset()
        K.si = 0
        wd = [sb("wd%d" % i, [128, 8, TB], BF16) for i in range(3)]
        ub = [sb("ub%d" % i, [128, 8, T], BF16) for i in range(2)]
        stf = [sb("stf5_%d" % i, [128, TB], F32) for i in range(4)]
        K.rtmp = [sb("rtmp5_%d" % i, [128, TB], F32) for i in range(2)]
        rh = resid_handler(l, 5, xT, stf, None)
        wfd = wfull[("w_down", l)]
        wk = ("wf", "w_down", l)
        pieces = [(nb, pc) for nb in range(8) for pc in range(16)]

        def pre(i):
            if i >= len(pieces):
                return
            nb, pc = pieces[i]
            ws, us = i % 3, i % 2
            wv = wfd[pc * 1024:(pc + 1) * 1024, nb * TB:(nb + 1) * TB].rearrange("(kc p) n -> p kc n", p=128)
            uv = uT[pc * 1024:(pc + 1) * 1024, :].rearrange("(kc p) t -> p kc t", p=128)
            P.dma("sp", wd[ws][:], wv, reads=[wk], writes=[("wd", ws)], key=("wd", ws))
            P.dma("sp", ub[us][:], uv, reads=["uT"], writes=[("ub", us)], key=("ub", us))
        pre(0)
        for i, (nb, pc) in enumerate(pieces):
            pre(i + 1)
            ws, us = i % 3, i % 2
            for kc in range(8):
                for m in range(4):
                    for tb in range(2):
                        bk = m * 2 + tb
                        P.op("pe", lambda e: e.matmul(ps[bk][:, :], wd[ws][:, kc, m * 128:(m + 1) * 128], ub[us][:, kc, tb * TB:(tb + 1) * TB],
                                                      start=(pc == 0 and kc == 0), stop=(pc == 15 and kc == 7)),
                             reads=[("wd", ws), ("ub", us)], writes=[psk[bk]])
            if pc == 15:
                for m in range(4):
                    for tb in range(2):
                        rh(m * 2 + tb, 128, tb, nb * TB + m * 128)
        P.barrier_all()

    def final_norm():
        work_reset()
        xs = [sb("fxs%d" % i, [128, TB], F32) for i in range(2)]
        sq = [sb("fsq%d" % i, [128, TB], F32) for i in range(2)]
        rstd = [sb("frstd%d" % i, [128, TB], F32) for i in range(2)]
        ot = [sb("fot%d" % i, [128, TB], F32) for i in range(2)]

        def loader(c, sl, tb):
            P.dma("sp", xs[sl][:], xT[c * 128:(c + 1) * 128, tb * TB:(tb + 1) * TB], reads=["xT"], writes=[("xs", sl)], key=("xs", sl))
            return ("xs", sl)
        for tb in range(2):
            rmsnorm_stats(loader, KC, D, rstd[tb], tb, xs, sq)
        for tb in range(2):
            for c in range(KC):
                sl = c % 2
                k = loader(c, sl, tb)
                P.op("dve", lambda e: e.scalar_tensor_tensor(out=ot[sl][:], in0=xs[sl][:], scalar=gvec[:, GV_FIN + c:GV_FIN + c + 1],
                                                             in1=rstd[tb][:], op0=ALU.mult, op1=ALU.mult),
                     reads=[k, "gvec", ("rstd", id(rstd[tb]))], writes=[("fot", sl)])
                P.dma("sp", outT[c * 128:(c + 1) * 128, tb * TB:(tb + 1) * TB], ot[sl][:], reads=[("fot", sl)], group="outT",
                      key=("st", "outT"))

    def dump(name, src, shape, dt, key):
        d = nc.dram_tensor(name, list(shape), dt, kind="ExternalOutput").ap()
        P.dma("sp", d, src, reads=[key], writes=[name], key=("dbg", name))
        dbg_out[name] = 1

    done = False
    for l in range(NL):
        if stage < 1:
            break
        phase1(l)
        if stage < 3:
            break
        phase2(l)
        if stage < 5:
            break
        phase3(l)
        if stage < 8:
            break
    else:
        done = True
    P.barrier_all()
    if done:
        final_norm()
    if "p1" in dbg:
        dump("dbg_qaT", qaT[:, :], [1024, T], BF16, "q1")
        dump("dbg_iqT", iqT[:, :], [2048, T], BF16, "q1")
        dump("dbg_qbnT", qbnT[:, :], [2048, T], BF16, "q1")
        dump("dbg_qbrT", qbrT[:, :], [1024, T], BF16, "q1")
        dump("dbg_qcT", qcT[:, :], [1024, T], BF16, "q1")
        dump("dbg_iw", iw_d[:, :], [T, 32], F32, "q1")
        dump("dbg_gk", gk_out[0:G_ROWS, :], [G_ROWS, T], BF16, "gk_out")
        dump("dbg_gf", gf_out, [8 * T, 8], F32, "gf_out")
        dump("dbg_gv", gv_out[0:GV_ROWS, :], [GV_ROWS, T], BF16, "gv_out")
    if "wf" in dbg:
        dump("dbg_wuq", wfull[("w_uq", 0)][:, :], [1024, 3072], BF16, ("wf", "w_uq", 0))
        dump("dbg_win", wfull[("w_in", 0)][0:D:64, 0:2048], [64, 2048], BF16, ("wf", "w_in", 0))
    if "act" in dbg:
        dump("dbg_actT", actT[:].rearrange("p k t -> p (k t)"), [128, KC * T], BF16, actk[0])
    if "xT" in dbg:
        dump("dbg_xT", xT[:, :], [D, T], F32, "xT")
    P.barrier_all()
    P.finish(["outT"] + list(dbg_out))
    K.dbg_out = dbg_out
    return nc, K


def _const_tables(j):
    ident = np.eye(128, dtype=np.float32)
    p = np.arange(128)
    R128 = np.zeros((128, 128), np.float32)
    R128[(p + 64) % 128, p] = 1.0
    R64 = np.zeros((128, 128), np.float32)
    part64 = (p // 64) * 64 + ((p % 64) + 32) % 64
    R64[part64, p] = 1.0
    triu = (p[:, None] <= p[None, :]).astype(np.float32)
    theta = 10000.0
    inv128 = np.exp(np.arange(0, 128, 2, dtype=np.float32) * np.float32(-np.log(theta) / 128)).astype(np.float32)
    inv64 = np.exp(np.arange(0, 64, 2, dtype=np.float32) * np.float32(-np.log(theta) / 64)).astype(np.float32)
    rtab = np.zeros((128, 4), np.float32)
    rtab[:, 0] = inv128[p % 64] / np.float32(2 * np.pi)
    rtab[:, 1] = inv64[p % 32] / np.float32(2 * np.pi)
    rtab[:, 2] = np.where(p < 64, -1.0, 1.0)
    rtab[:, 3] = np.where((p % 64) < 32, -1.0, 1.0)
    sel = np.zeros((128, 8), np.float32)
    sel[:, j] = 1.0
    cst = np.concatenate([ident, R128, R64, triu, rtab, sel], axis=1).astype(np.float32)
    q = np.arange(128)[:, None]
    s = np.arange(128)[None, :]
    chunk = (s // 64) <= (q // 64)
    frame = s <= q
    maskB = np.zeros((128, 2, 8, 128), np.float32)
    maskQ = np.zeros((128, 8, 128), np.float32)
    for jk in range(8):
        if jk < j:
            continue
        if jk == j:
            maskB[:, 0, jk, :] = np.where(chunk, 0.0, NEGB)
            maskB[:, 1, jk, :] = np.where(frame, 0.0, NEGB)
            maskQ[:, jk, :] = np.where(chunk, 0.0, -1.0e30)
        else:
            maskB[:, :, jk, :] = NEGB
            maskQ[:, jk, :] = -1.0e30
    return cst, maskB.reshape(128, -1), maskQ.reshape(128, -1)


def prep_inputs(inp, NL=2, stage=99):
    x = np.asarray(inp["x"], np.float32)
    pos = np.asarray(inp["positions"], np.int32)
    c = np.asarray(inp["c"], np.float32)
    cT = np.ascontiguousarray(c.reshape(2, 32, 128).transpose(2, 1, 0)).reshape(128, 64)
    gvec = np.zeros((128, 512), np.float32)

    def fm(v):
        return np.asarray(v, np.float32).reshape(-1, 128).T
    for l in range(NL):
        gvec[:, 0 + l * 32:0 + (l + 1) * 32] = fm(inp["g_attn"][l])
        gvec[:, 64 + l * 32:64 + (l + 1) * 32] = fm(inp["g_mlp"][l])
        gvec[:, 160 + l * 8:160 + (l + 1) * 8] = fm(inp["g_cq"][l])
        gvec[:, 176 + l * 4:176 + (l + 1) * 4] = fm(inp["g_ckv"][l])
        gvec[:, 184 + l * 32:184 + (l + 1) * 32] = fm(np.concatenate([inp["g_out_a"][l], inp["g_out_b"][l], inp["g_out_c"][l]]))
    gvec[:, 128:160] = fm(inp["g_final"])
    bfrow = np.ascontiguousarray(np.broadcast_to(np.asarray(inp["b_f"], np.float32)[:NL].reshape(1, NL * 8), (128, NL * 8)))
    maps = []
    for j in range(NC):
        tiles = []
        ptiles = []
        for b in range(2):
            for g in range(4):
                t0 = (8 * g + j) * 128
                tiles.append(x[b, t0:t0 + 128, :])
                ptiles.append(pos[b, t0:t0 + 128])
        xl = np.concatenate(tiles, axis=0)
        cst, maskB, maskQ = _const_tables(j)
        m = {
            "xT_in": np.ascontiguousarray(xl.T),
            "pos": np.concatenate(ptiles).reshape(1, T).astype(np.int32),
            "cT": cT,
            "wada": np.ascontiguousarray(np.asarray(inp["w_ada"])[:NL, :, j * 3072:(j + 1) * 3072]),
            "bada": np.ascontiguousarray(np.asarray(inp["b_ada"], np.float32)[:NL, j * 3072:(j + 1) * 3072].reshape(NL * 24, 128).T),
            "gvec": gvec, "bfrow": bfrow, "cst": cst, "maskB": maskB, "maskQ": maskQ,
        }
        for n, rows in (("w_in", D), ("w_uq", 1024), ("w_ukv", 512), ("w_out", D), ("w_up", D), ("w_down", DFF)):
            if n not in wnames_for(stage):
                continue
            r = rows // 8
            m[n] = np.ascontiguousarray(np.asarray(inp[n])[:NL, j * r:(j + 1) * r, :])
        maps.append(m)
    return maps


def assemble(results, name="outT"):
    out = np.zeros((2, 4096, D), np.float32)
    for j in range(NC):
        o = np.asarray(results[j][name]).T
        for b in range(2):
            for g in range(4):
                t0 = (8 * g + j) * 128
                lt = b * 4 + g
                out[b, t0:t0 + 128, :] = o[lt * 128:(lt + 1) * 128, :]
    return out


_CACHE = {}


def kernel(**inputs):
    if "nc" not in _CACHE:
        _CACHE["nc"] = build(NL=2)[0]
    maps = prep_inputs(inputs, NL=2)
    res = run_bass_kernel_spmd(_CACHE["nc"], maps, core_ids=list(range(NC)))
    return assemble(res.results)
```
